# Optimizing a Trainium2 kernel written in Bass

```python
import jax
import jax.numpy as jnp
from jax import lax
import numpy as np

D_MODEL = 4096
BATCH = 4
SEQ = 2048
DEPTH = 2

CTX_LEN = 256
GRID_W = 64
HEAD_DIM = 128
MIX_WIDTH = D_MODEL
LRU_WIDTH = MIX_WIDTH // 2
LRU_BLOCKS = LRU_WIDTH // HEAD_DIM
LRU_BLOCK = LRU_WIDTH // LRU_BLOCKS
LRU_C = 8.0
CONV_K = 4
CONV_LEFT = 2
RET_HEADS = (MIX_WIDTH // 2) // HEAD_DIM
RET_WIDTH = RET_HEADS * HEAD_DIM
RET_CHUNK = 128
RET_DECAY_MIN_EXP = 5.0
RET_DECAY_SPAN = 7.0
AB_SPLITS = [LRU_WIDTH, 2 * LRU_WIDTH, 2 * LRU_WIDTH + RET_WIDTH, 2 * LRU_WIDTH + 2 * RET_WIDTH, 2 * LRU_WIDTH + 3 * RET_WIDTH]
AB_IN = 2 * LRU_WIDTH + 4 * RET_WIDTH
GQA_Q_HEADS = (MIX_WIDTH // 2) // HEAD_DIM
GQA_KV_HEADS = GQA_Q_HEADS // 4
NA_HEADS = (MIX_WIDTH // 2) // HEAD_DIM
NA_WIN_ROWS = 8
NA_WIN_COLS = 16
Q_BLOCK = 128
CD_Q_COLS = (GQA_Q_HEADS + NA_HEADS) * HEAD_DIM
CD_IN = CD_Q_COLS + (2 * GQA_KV_HEADS + 2 * NA_HEADS) * HEAD_DIM
ROPE_THETA = 10000.0
N_EXPERTS = 16
EXPERT_FF = D_MODEL // 4
EC_CAPACITY = 2
EPS = 1e-6

kernel_name = 'hybrid_lru_retention_gqa_natten_ecmoe_diffusion'


def rms_norm(x, gain=None):
    xf = x.astype(jnp.float32)
    y = xf * lax.rsqrt(jnp.mean(xf * xf, axis=-1, keepdims=True) + EPS)
    if gain is not None:
        y = y * gain.astype(jnp.float32)
    return y.astype(x.dtype)


def modulation(cvec, w, b, n_chunks):
    cols = n_chunks * D_MODEL
    m = jax.nn.silu(cvec) @ w[:, :cols] + b[:cols]
    return jnp.split(m, n_chunks, axis=-1)


def axial_rope(n_tok):
    t = jnp.arange(n_tok)
    row = (t // GRID_W).astype(jnp.float32)
    col = (t % GRID_W).astype(jnp.float32)
    n_freq = HEAD_DIM // 4
    inv = ROPE_THETA ** (-jnp.arange(n_freq, dtype=jnp.float32) / n_freq)
    ang = jnp.concatenate([row[:, None] * inv, col[:, None] * inv], axis=-1)
    return jnp.cos(ang), jnp.sin(ang)


def apply_rope(x, cos, sin):
    xf = x.astype(jnp.float32).reshape(*x.shape[:-1], HEAD_DIM // 2, 2)
    x1, x2 = xf[..., 0], xf[..., 1]
    c, s = cos[:, None, :], sin[:, None, :]
    y = jnp.stack([x1 * c - x2 * s, x1 * s + x2 * c], axis=-1)
    return y.reshape(x.shape).astype(x.dtype)


def split_heads(z, head_counts):
    offs = [int(o) * HEAD_DIM for o in np.cumsum(head_counts)[:-1]]
    parts = jnp.split(z, offs, axis=-1)
    return [p.reshape(p.shape[0], p.shape[1], n, HEAD_DIM) for p, n in zip(parts, head_counts)]


def centred_dwconv(x, w, b):
    y = lax.conv_general_dilated(x, w[:, None, :], window_strides=(1,),
                                 padding=[(CONV_LEFT, CONV_K - 1 - CONV_LEFT)],
                                 dimension_numbers=('NWC', 'WIO', 'NWC'),
                                 feature_group_count=x.shape[-1])
    return y + b


def rglru_coeffs(u, wa, ba, wx, bx, lam):
    ub = u.reshape(u.shape[0], u.shape[1], LRU_BLOCKS, LRU_BLOCK)
    r = jax.nn.sigmoid(jnp.einsum('blhi,hij->blhj', ub, wa).reshape(u.shape) + ba)
    i = jax.nn.sigmoid(jnp.einsum('blhi,hij->blhj', ub, wx).reshape(u.shape) + bx)
    log_a = -LRU_C * r * jax.nn.softplus(-lam)
    a = jnp.exp(log_a)
    b = jnp.sqrt(-jnp.expm1(2.0 * log_a)) * (i * u)
    return a, b


def linear_scan(a, b, h0, reverse):
    i0 = -1 if reverse else 0
    b = b.at[:, i0].add(a[:, i0] * h0)

    def combine(early, late):
        return late[0] * early[0], late[0] * early[1] + late[1]

    _, h = lax.associative_scan(combine, (a, b), reverse=reverse, axis=1)
    return h


def retention_log_decay(reverse_heads):
    e = RET_DECAY_MIN_EXP + RET_DECAY_SPAN * jnp.arange(RET_HEADS, dtype=jnp.float32) / (RET_HEADS - 1)
    if reverse_heads:
        e = e[::-1]
    return jnp.log1p(-jnp.exp2(-e))


def retention_scan(q, k, v, log_g, s0, inclusive):
    bsz, length, nh, dh = q.shape
    n_chunk = length // RET_CHUNK

    def to_chunks(t):
        return t.reshape(bsz, n_chunk, RET_CHUNK, nh, dh).transpose(1, 0, 3, 2, 4)

    pos = jnp.arange(RET_CHUNK, dtype=jnp.float32)
    diff = pos[:, None] - pos[None, :]
    mask = (diff >= 0) if inclusive else (diff > 0)
    d_intra = jnp.where(mask, jnp.exp(jnp.maximum(diff, 0.0) * log_g[:, None, None]), 0.0)
    q_decay = jnp.exp((pos + 1.0) * log_g[:, None])[..., None]
    k_decay = jnp.exp((RET_CHUNK - 1.0 - pos) * log_g[:, None])[..., None]
    chunk_decay = jnp.exp(RET_CHUNK * log_g)[:, None, None]

    def step(s, qkv):
        qi, ki, vi = qkv
        scores = jnp.einsum('bhid,bhjd->bhij', qi, ki) * d_intra
        o = jnp.einsum('bhij,bhjd->bhid', scores, vi) + jnp.einsum('bhid,bhde->bhie', qi * q_decay, s)
        s = s * chunk_decay + jnp.einsum('bhjd,bhje->bhde', ki * k_decay, vi)
        return s, o

    s_fin, o = lax.scan(step, s0, (to_chunks(q), to_chunks(k), to_chunks(v)))
    return o.transpose(1, 0, 3, 2, 4).reshape(bsz, length, nh, dh), s_fin


def bidir_retention(qc, kc, vc, ql, kl, vl):
    s0 = jnp.zeros((qc.shape[0], RET_HEADS, HEAD_DIM, HEAD_DIM), jnp.float32)
    out_c, out_l = 0.0, 0.0
    for rev in (False, True):
        log_g = retention_log_decay(rev)
        flip = (lambda t: t[:, ::-1]) if rev else (lambda t: t)
        o_c, s_c = retention_scan(flip(qc), flip(kc), flip(vc), log_g, s0, not rev)
        o_l, _ = retention_scan(flip(ql), flip(kl), flip(vl), log_g, s_c, not rev)
        out_c = out_c + flip(o_c)
        out_l = out_l + flip(o_l)
    return out_c, out_l


def mixer_ab(hc, hl, w_in, conv_w, conv_b, ra_w, ra_b, ix_w, ix_b, lam, w_out, cos, sin):
    f32 = jnp.float32
    dt = hl.dtype
    xa_c, ya_c, q_c, k_c, v_c, g_c = jnp.split(hc @ w_in, AB_SPLITS, axis=-1)
    xa_l, ya_l, q_l, k_l, v_l, g_l = jnp.split(hl @ w_in, AB_SPLITS, axis=-1)

    u_c = centred_dwconv(xa_c, conv_w, conv_b).astype(f32)
    u_l = centred_dwconv(xa_l, conv_w, conv_b).astype(f32)
    h_c_sum, h_l_sum = 0.0, 0.0
    for d, rev in enumerate((False, True)):
        prm = [t.astype(f32) for t in (ra_w[d], ra_b[d], ix_w[d], ix_b[d], lam[d])]
        a, b = rglru_coeffs(u_c, *prm)
        h_c = linear_scan(a, b, jnp.zeros_like(u_c[:, 0]), rev)
        h_end = h_c[:, 0] if rev else h_c[:, -1]
        a, b = rglru_coeffs(u_l, *prm)
        h_l = linear_scan(a, b, h_end, rev)
        h_c_sum = h_c_sum + h_c
        h_l_sum = h_l_sum + h_l
    ya_c_out = (h_c_sum * jax.nn.gelu(ya_c.astype(f32))).astype(dt)
    ya_l_out = (h_l_sum * jax.nn.gelu(ya_l.astype(f32))).astype(dt)

    scale = HEAD_DIM ** -0.5

    def heads(t):
        return t.reshape(t.shape[0], t.shape[1], RET_HEADS, HEAD_DIM).astype(f32)

    qr_l = apply_rope(heads(q_l), cos, sin) * scale
    kr_l = apply_rope(heads(k_l), cos, sin)
    r_c, r_l = bidir_retention(heads(q_c) * scale, heads(k_c), heads(v_c), qr_l, kr_l, heads(v_l))
    yb_c = (rms_norm(r_c).reshape(r_c.shape[0], r_c.shape[1], RET_WIDTH) * jax.nn.silu(g_c.astype(f32))).astype(dt)
    yb_l = (rms_norm(r_l).reshape(r_l.shape[0], r_l.shape[1], RET_WIDTH) * jax.nn.silu(g_l.astype(f32))).astype(dt)

    o_c = jnp.concatenate([ya_c_out, yb_c], axis=-1) @ w_out
    o_l = jnp.concatenate([ya_l_out, yb_l], axis=-1) @ w_out
    return o_c, o_l


def gqa_blocks(q, k, v):
    bsz, seq, hq, dh = q.shape
    hkv = k.shape[2]
    grp = hq // hkv
    nb = seq // Q_BLOCK
    qb = q.reshape(bsz, nb, Q_BLOCK, hkv, grp, dh).transpose(1, 0, 2, 3, 4, 5)
    scale = dh ** -0.5

    def block(qi):
        s = jnp.einsum('bqhgd,bkhd->bhgqk', qi, k, preferred_element_type=jnp.float32) * scale
        p = jax.nn.softmax(s, axis=-1).astype(v.dtype)
        return jnp.einsum('bhgqk,bkhd->bqhgd', p, v)

    o = lax.map(block, qb)
    return o.transpose(1, 0, 2, 3, 4, 5).reshape(bsz, seq, hq * dh)


def neighbourhood_index(rows):
    kh, kw = min(NA_WIN_ROWS, rows), NA_WIN_COLS
    n = rows * GRID_W
    t = jnp.arange(n)
    r, col = t // GRID_W, t % GRID_W
    r0 = jnp.clip(r - kh // 2, 0, rows - kh)
    c0 = jnp.clip(col - kw // 2, 0, GRID_W - kw)
    kr = r0[:, None] + jnp.arange(kh)[None, :]
    kc = c0[:, None] + jnp.arange(kw)[None, :]
    key_idx = (kr[:, :, None] * GRID_W + kc[:, None, :]).reshape(n, kh * kw)
    rel_r = jnp.broadcast_to((kr - r[:, None])[:, :, None], (n, kh, kw)).reshape(n, kh * kw) + NA_WIN_ROWS - 1
    rel_c = jnp.broadcast_to((kc - col[:, None])[:, None, :], (n, kh, kw)).reshape(n, kh * kw) + NA_WIN_COLS - 1
    return key_idx, rel_r, rel_c


def neighbourhood_attention(q, k, v, k_ctx, v_ctx, rpb, rows):
    bsz, seq, nh, dh = q.shape
    key_idx, rel_r, rel_c = neighbourhood_index(rows)
    n_local = key_idx.shape[1]
    scale = dh ** -0.5

    def per_row(t):
        return t.reshape(rows, GRID_W, n_local)

    qb = q.reshape(bsz, rows, GRID_W, nh, dh).transpose(1, 0, 2, 3, 4)

    def block(args):
        qi, idx, ri, ci = args
        kb = jnp.take(k, idx, axis=1)
        vb = jnp.take(v, idx, axis=1)
        s_loc = jnp.einsum('bqhd,bqnhd->bhqn', qi, kb, preferred_element_type=jnp.float32) * scale
        s_loc = s_loc + rpb[:, ri, ci].astype(jnp.float32)
        s_ctx = jnp.einsum('bqhd,bkhd->bhqk', qi, k_ctx, preferred_element_type=jnp.float32) * scale
        p = jax.nn.softmax(jnp.concatenate([s_loc, s_ctx], axis=-1), axis=-1).astype(v.dtype)
        return (jnp.einsum('bhqn,bqnhd->bqhd', p[..., :n_local], vb)
                + jnp.einsum('bhqk,bkhd->bqhd', p[..., n_local:], v_ctx))

    o = lax.map(block, (qb, per_row(key_idx), per_row(rel_r), per_row(rel_c)))
    return o.transpose(1, 0, 2, 3, 4).reshape(bsz, seq, nh * dh)


def mixer_cd(hc, hl, w_in, gq_qn, gq_kn, na_qn, na_kn, rpb, w_out, cos, sin):
    rows = hl.shape[1] // GRID_W
    gq, nq, gk, gv, nk, nv = split_heads(hl @ w_in, [GQA_Q_HEADS, NA_HEADS, GQA_KV_HEADS, GQA_KV_HEADS, NA_HEADS, NA_HEADS])
    gk_c, gv_c, nk_c, nv_c = split_heads(hc @ w_in[:, CD_Q_COLS:], [GQA_KV_HEADS, GQA_KV_HEADS, NA_HEADS, NA_HEADS])
    gq = apply_rope(rms_norm(gq, gq_qn), cos, sin)
    gk = apply_rope(rms_norm(gk, gq_kn), cos, sin)
    keys = jnp.concatenate([rms_norm(gk_c, gq_kn), gk], axis=1)
    vals = jnp.concatenate([gv_c, gv], axis=1)
    o_gqa = gqa_blocks(gq, keys, vals)
    o_na = neighbourhood_attention(rms_norm(nq, na_qn), rms_norm(nk, na_kn), nv,
                                   rms_norm(nk_c, na_kn), nv_c, rpb, rows)
    return jnp.concatenate([o_gqa, o_na], axis=-1) @ w_out


def expert_choice_ffn(h, router, w1, w3, w2):
    bsz, n, _ = h.shape
    cap = EC_CAPACITY * n // N_EXPERTS
    aff = jax.nn.softmax(jnp.einsum('bnd,de->bne', h, router, preferred_element_type=jnp.float32), axis=-1)
    gate, tok = lax.top_k(jnp.swapaxes(aff, 1, 2), cap)
    bidx = jnp.arange(bsz)[:, None, None]
    xe = h[bidx, tok]
    hid = jax.nn.silu(jnp.einsum('becd,edf->becf', xe, w1)) * jnp.einsum('becd,edf->becf', xe, w3)
    ye = jnp.einsum('becf,efd->becd', hid, w2) * gate[..., None].astype(h.dtype)
    return jnp.zeros_like(h).at[bidx, tok].add(ye)


def setup_inputs(seed: int = 0) -> dict:
    key = jax.random.key(seed)
    kit = iter(jax.random.split(key, 64))
    f32 = jnp.float32
    d, ff = D_MODEL, EXPERT_FF

    def nrm(shape, std):
        return std * jax.random.normal(next(kit), shape, f32)

    def gain(n):
        return 1.0 + nrm((n,), 0.01)

    def lru_lambda():
        u = jax.random.uniform(next(kit), (2, LRU_WIDTH), f32, 0.9, 0.999)
        a = u ** (1.0 / LRU_C)
        return jnp.log(a) - jnp.log1p(-a)

    inp = {
        'x': nrm((BATCH, SEQ, d), 1.0),
        'c': nrm((BATCH, d), 1.0),
        'ctx': nrm((BATCH, CTX_LEN, d), 1.0),
        'c_ctx': nrm((d,), 1.0),
    }
    for li in range(DEPTH):
        p = f'l{li}_'
        inp[p + 'mod_w'] = nrm((d, 6 * d), 0.5 * d ** -0.5)
        inp[p + 'mod_b'] = nrm((6 * d,), 0.01)
        inp[p + 'norm1'] = gain(d)
        inp[p + 'norm2'] = gain(d)
        if li % 2 == 0:
            inp[p + 'w_in'] = nrm((d, AB_IN), d ** -0.5)
            inp[p + 'conv_w'] = nrm((CONV_K, LRU_WIDTH), CONV_K ** -0.5)
            inp[p + 'conv_b'] = nrm((LRU_WIDTH,), 0.01)
            inp[p + 'lru_ra_w'] = nrm((2, LRU_BLOCKS, LRU_BLOCK, LRU_BLOCK), LRU_BLOCK ** -0.5)
            inp[p + 'lru_ra_b'] = nrm((2, LRU_WIDTH), 0.01)
            inp[p + 'lru_ix_w'] = nrm((2, LRU_BLOCKS, LRU_BLOCK, LRU_BLOCK), LRU_BLOCK ** -0.5)
            inp[p + 'lru_ix_b'] = nrm((2, LRU_WIDTH), 0.01)
            inp[p + 'lru_lambda'] = lru_lambda()
        else:
            inp[p + 'w_in'] = nrm((d, CD_IN), d ** -0.5)
            inp[p + 'gqa_q_norm'] = gain(HEAD_DIM)
            inp[p + 'gqa_k_norm'] = gain(HEAD_DIM)
            inp[p + 'na_q_norm'] = gain(HEAD_DIM)
            inp[p + 'na_k_norm'] = gain(HEAD_DIM)
            inp[p + 'na_rpb'] = nrm((NA_HEADS, 2 * NA_WIN_ROWS - 1, 2 * NA_WIN_COLS - 1), 0.02)
        inp[p + 'w_out'] = nrm((MIX_WIDTH, d), MIX_WIDTH ** -0.5)
        inp[p + 'router'] = nrm((d, N_EXPERTS), d ** -0.5)
        inp[p + 'exp_w1'] = nrm((N_EXPERTS, d, ff), d ** -0.5)
        inp[p + 'exp_w3'] = nrm((N_EXPERTS, d, ff), d ** -0.5)
        inp[p + 'exp_w2'] = nrm((N_EXPERTS, ff, d), ff ** -0.5)
    return inp


def reference(x, c, ctx, c_ctx,
              l0_mod_w, l0_mod_b, l0_norm1, l0_norm2, l0_w_in, l0_conv_w, l0_conv_b,
              l0_lru_ra_w, l0_lru_ra_b, l0_lru_ix_w, l0_lru_ix_b, l0_lru_lambda, l0_w_out,
              l0_router, l0_exp_w1, l0_exp_w3, l0_exp_w2,
              l1_mod_w, l1_mod_b, l1_norm1, l1_norm2, l1_w_in, l1_gqa_q_norm, l1_gqa_k_norm,
              l1_na_q_norm, l1_na_k_norm, l1_na_rpb, l1_w_out,
              l1_router, l1_exp_w1, l1_exp_w3, l1_exp_w2):
    cos, sin = axial_rope(x.shape[1])
    layers = [
        dict(mod_w=l0_mod_w, mod_b=l0_mod_b, norm1=l0_norm1, norm2=l0_norm2,
             router=l0_router, w1=l0_exp_w1, w3=l0_exp_w3, w2=l0_exp_w2,
             mixer=lambda hc, hl: mixer_ab(hc, hl, l0_w_in, l0_conv_w, l0_conv_b, l0_lru_ra_w, l0_lru_ra_b,
                                           l0_lru_ix_w, l0_lru_ix_b, l0_lru_lambda, l0_w_out, cos, sin)),
        dict(mod_w=l1_mod_w, mod_b=l1_mod_b, norm1=l1_norm1, norm2=l1_norm2,
             router=l1_router, w1=l1_exp_w1, w3=l1_exp_w3, w2=l1_exp_w2,
             mixer=lambda hc, hl: (None, mixer_cd(hc, hl, l1_w_in, l1_gqa_q_norm, l1_gqa_k_norm,
                                                  l1_na_q_norm, l1_na_k_norm, l1_na_rpb, l1_w_out, cos, sin))),
    ]
    xl, xc = x, ctx
    for li in range(DEPTH):
        p = layers[li]
        last = li == DEPTH - 1
        sh1, sc1, g1, sh2, sc2, g2 = [m[:, None, :] for m in modulation(c, p['mod_w'], p['mod_b'], 6)]
        cm = modulation(c_ctx, p['mod_w'], p['mod_b'], 2 if last else 6)
        hl = rms_norm(xl, p['norm1']) * (1.0 + sc1) + sh1
        hc = rms_norm(xc, p['norm1']) * (1.0 + cm[1]) + cm[0]
        oc, ol = p['mixer'](hc, hl)
        xl = xl + g1 * ol
        hl = rms_norm(xl, p['norm2']) * (1.0 + sc2) + sh2
        xl = xl + g2 * expert_choice_ffn(hl, p['router'], p['w1'], p['w3'], p['w2'])
        if not last:
            xc = xc + cm[2] * oc
            hc = rms_norm(xc, p['norm2']) * (1.0 + cm[4]) + cm[3]
            xc = xc + cm[5] * expert_choice_ffn(hc, p['router'], p['w1'], p['w3'], p['w2'])
    return xl
```

```python
import numpy as np
from contextlib import ExitStack
import concourse.bass as bass
import concourse.mybir as mybir
from concourse.bass_utils import run_bass_kernel_spmd

F32 = mybir.dt.float32
BF16 = mybir.dt.bfloat16
I32 = mybir.dt.int32
U32 = mybir.dt.uint32
AF = mybir.ActivationFunctionType
ALU = mybir.AluOpType
AX = mybir.AxisListType

D = 4096
NCORES = 8


class Dep:
    __slots__ = ("w", "r", "sem", "semv", "name")

    def __init__(self, name=""):
        self.w = None
        self.r = []
        self.sem = None
        self.semv = 0
        self.name = name


class T:
    def __init__(self, k, ap_src, name):
        self.k = k
        self.t = ap_src
        self.dep = Dep(name)
        self.name = name

    def __getitem__(self, idx):
        return self.t[idx]


class Eng:
    def __init__(self, name, be, sem):
        self.name = name
        self.be = be
        self.sem = sem
        self.count = 0
        self.seen = {}
        self.ops = []
        self.pending_noinc = False


class K:
    def __init__(self):
        self.nc = bass.Bass("TRN2", target_bir_lowering=False)
        self.es = ExitStack()
        nc = self.nc
        self.engs = {}
        for name, be in (("pe", nc.tensor), ("act", nc.scalar), ("dve", nc.vector),
                         ("pool", nc.gpsimd), ("sp", nc.sync)):
            sem = self.es.enter_context(nc.semaphore("s_" + name))
            self.engs[name] = Eng(name, be, sem)
        self.sem_deps = []
        self.free_sems = []
        self.nsem = 0
        self.out_events = []

    def dram(self, name, shape, dt, kind):
        t = self.nc.dram_tensor(name, list(shape), dt, kind=kind)
        return T(self, t.ap(), name)

    def sbuf(self, name, shape, dt):
        t = self.es.enter_context(self.nc.sbuf_tensor(name, list(shape), dt))
        return T(self, t, name)

    def psum(self, name, shape, dt=F32):
        t = self.es.enter_context(self.nc.psum_tensor(name, list(shape), dt))
        return T(self, t, name)

    def view(self, base, ap, name):
        v = T(self, ap, name)
        return v

    def _deps(self, reads, writes):
        evs = []
        for d in reads:
            d = d.dep if isinstance(d, T) else d
            if d.w is not None:
                evs.append(d.w)
        for d in writes:
            d = d.dep if isinstance(d, T) else d
            if d.w is not None:
                evs.append(d.w)
            evs.extend(d.r)
        return evs

    def _commit(self, reads, writes, ev):
        for d in reads:
            d = d.dep if isinstance(d, T) else d
            d.r.append(ev)
            if len(d.r) > 64:
                best = {}
                for s, v in d.r:
                    if id(s) not in best or best[id(s)][1] < v:
                        best[id(s)] = (s, v)
                d.r = list(best.values())
        for d in writes:
            d = d.dep if isinstance(d, T) else d
            d.w = ev
            d.r = []

    def _waits(self, e, evs, skip_self=False):
        need = {}
        for s, v in evs:
            if skip_self and s is e.sem:
                continue
            if e.seen.get(id(s), 0) >= v:
                continue
            if id(s) not in need or need[id(s)][1] < v:
                need[id(s)] = (s, v)
        for s, v in need.values():
            e.seen[id(s)] = v
        return list(need.values())

    def op(self, eng, fn, reads=(), writes=(), inc=True, skip_self=False):
        e = self.engs[eng]
        evs = self._deps(reads, writes)
        waits = self._waits(e, evs, skip_self=skip_self or eng == "pe")
        if inc:
            e.count += 1
            ev = (e.sem, e.count)
            e.pending_noinc = False
        else:
            ev = (e.sem, e.count + 1)
            e.pending_noinc = True
        e.ops.append((waits, fn, (e.sem, 1) if inc else None))
        self._commit(reads, writes, ev)
        return ev

    def dma(self, eng, out, in_, reads=(), writes=(), key=None, is_output=False):
        e = self.engs[eng]
        kd = key.dep if isinstance(key, T) else key
        if kd.sem is None:
            if self.free_sems:
                kd.sem, kd.semv = self.free_sems.pop()
            else:
                kd.sem = self.es.enter_context(self.nc.semaphore("d%d" % self.nsem))
                self.nsem += 1
            self.sem_deps.append(kd)
        evs = self._deps(reads, writes)
        waits = self._waits(e, evs)
        kd.semv += 16
        ev = (kd.sem, kd.semv)

        def fn(be, out=out, in_=in_):
            return be.dma_start(out=out, in_=in_)
        e.ops.append((waits, fn, (kd.sem, 16)))
        self._commit(reads, writes, ev)
        if is_output:
            self.out_events.append(ev)
        return ev

    def idma(self, out, in_, out_offset=None, in_offset=None, reads=(), writes=(), key=None, add=False, is_output=False):
        e = self.engs["pool"]
        kd = key.dep if isinstance(key, T) else key
        if kd.sem is None:
            if self.free_sems:
                kd.sem, kd.semv = self.free_sems.pop()
            else:
                kd.sem = self.es.enter_context(self.nc.semaphore("d%d" % self.nsem))
                self.nsem += 1
            self.sem_deps.append(kd)
        evs = self._deps(reads, writes)
        waits = self._waits(e, evs)
        kd.semv += 16
        ev = (kd.sem, kd.semv)

        def fn(be):
            kw = {}
            if add:
                kw["compute_op"] = ALU.add
            return be.indirect_dma_start(out=out, out_offset=out_offset, in_=in_, in_offset=in_offset, **kw)
        e.ops.append((waits, fn, (kd.sem, 16)))
        self._commit(reads, writes, ev)
        if is_output:
            self.out_events.append(ev)
        return ev

    def act(self, out, in_, func, r, w, eng="act", ss=False, **kw):
        return self.op(eng, lambda be: be.activation(out=out, in_=in_, func=func, **kw), r, w, skip_self=ss)

    def tt(self, eng, out, in0, in1, op, r, w):
        return self.op(eng, lambda be: be.tensor_tensor(out=out, in0=in0, in1=in1, op=op), r, w)

    def ts(self, eng, out, in0, s1, s2, op0, op1, r, w, **kw):
        if op1 is None:
            return self.op(eng, lambda be: be.tensor_scalar(out=out, in0=in0, scalar1=s1, scalar2=None, op0=op0, **kw), r, w)
        return self.op(eng, lambda be: be.tensor_scalar(out=out, in0=in0, scalar1=s1, scalar2=s2, op0=op0, op1=op1, **kw), r, w)

    def stt(self, out, in0, scalar, in1, op0, op1, r, w, eng="dve"):
        return self.op(eng, lambda be: be.scalar_tensor_tensor(out=out, in0=in0, scalar=scalar, in1=in1, op0=op0, op1=op1), r, w)

    def copy(self, eng, out, in_, r, w):
        if eng == "act":
            return self.op(eng, lambda be: be.copy(out=out, in_=in_), r, w)
        return self.op(eng, lambda be: be.tensor_copy(out=out, in_=in_), r, w)

    def mm(self, out, lhsT, rhs, start, stop, r, w, inc=True):
        return self.op("pe", lambda be: be.matmul(out, lhsT, rhs, start=start, stop=stop), r, w, inc=inc)

    def tr(self, out, in_, ident, r, w, inc=True):
        return self.op("pe", lambda be: be.transpose(out, in_, ident), r, w, inc=inc)

    def memset(self, eng, out, val, w):
        return self.op(eng, lambda be: be.memset(out, val), (), w)

    def carve(self, arena, off, n, dt, name):
        words = n if dt in (F32, I32, U32) else (n + 1) // 2
        ap = arena.t[:, off:off + words]
        if dt not in (F32,):
            ap = ap.bitcast(dt)
        return T(self, ap, name), off + words

    def barrier(self):
        evs = []
        for e in self.engs.values():
            if e.pending_noinc:
                self.op(e.name, lambda be: be.nop(), inc=True)
            if e.count:
                evs.append((e.sem, e.count))
        evs.extend((d.sem, d.semv) for d in self.sem_deps if d.semv)
        for e in self.engs.values():
            waits = self._waits(e, evs, skip_self=True)
            if waits:
                e.ops.append((waits, None, None))
        for d in self.sem_deps:
            self.free_sems.append((d.sem, d.semv))
            d.sem = None
            d.semv = 0
        self.sem_deps = []

    def finish(self):
        for e in self.engs.values():
            if e.pending_noinc:
                self.op(e.name, lambda be: be.nop(), inc=True)
        sp = self.engs["sp"]
        waits = self._waits(sp, self.out_events)
        if waits:
            sp.ops.append((waits, None, None))
        nc = self.nc
        with nc.Block() as block:
            for name, reg in (("pe", block.tensor), ("act", block.scalar), ("dve", block.vector),
                              ("pool", block.gpsimd), ("sp", block.sync)):
                e = self.engs[name]

                def body(be, e=e):
                    for waits, fn, inc in e.ops:
                        for s, v in waits:
                            be.wait_ge(s, v)
                        if fn is not None:
                            ins = fn(be)
                            if inc is not None:
                                ins.then_inc(inc[0], inc[1])
                reg(body)
        self.es.close()
        return nc


MCOLS = 6 * D // NCORES


def build_mod():
    k = K()
    csT = k.dram("csT", [128, 32 * 8], F32, "ExternalInput")
    mw = k.dram("mw", [2, 4096, MCOLS], F32, "ExternalInput")
    mb = k.dram("mb", [2, 8, MCOLS], F32, "ExternalInput")
    out = k.dram("mod", [2, 8, MCOLS], F32, "ExternalOutput")
    c_sb = k.sbuf("c_sb", [128, 256], F32)
    s_sb = k.sbuf("s_sb", [128, 256], F32)
    b_sb = [k.sbuf("b_sb%d" % l, [8, MCOLS], F32) for l in range(2)]
    r_sb = [k.sbuf("r_sb%d" % l, [8, MCOLS], F32) for l in range(2)]
    NB = 4
    wb = [k.sbuf("wb%d" % i, [128, MCOLS], F32) for i in range(NB)]
    ps = [k.psum("ps%d" % i, [128, 512]) for i in range(6)]
    k.dma("sp", c_sb[:], csT[:], writes=[c_sb], key=c_sb)
    for l in range(2):
        k.dma("pool", b_sb[l][:], mb[l], writes=[b_sb[l]], key=b_sb[l])
    k.op("act", lambda be: be.activation(out=s_sb[:], in_=c_sb[:], func=AF.Silu), reads=[c_sb], writes=[s_sb])
    it = 0
    for l in range(2):
        for kt in range(32):
            w = wb[it % NB]
            it += 1
            k.dma("sp" if it % 2 else "pool", w[:], mw[l, kt * 128:(kt + 1) * 128, :], writes=[w], key=w)
            for n in range(6):
                k.op("pe", lambda be, n=n, kt=kt, w=w: be.matmul(
                    ps[n][0:8, :], s_sb[:, kt * 8:(kt + 1) * 8], w[:, n * 512:(n + 1) * 512],
                    start=(kt == 0), stop=(kt == 31)),
                    reads=[s_sb, w], writes=[ps[n]], inc=(n == 5))
        for n in range(6):
            k.op("dve", lambda be, n=n, l=l: be.tensor_tensor(
                out=r_sb[l][:, n * 512:(n + 1) * 512], in0=ps[n][0:8, :], in1=b_sb[l][:, n * 512:(n + 1) * 512],
                op=ALU.add), reads=[ps[n], b_sb[l]], writes=[r_sb[l]])
        k.dma("sp", out[l], r_sb[l][:], reads=[r_sb[l]], key=r_sb[l], is_output=True)
    return k.finish()


def run_mod(inp):
    cs = np.zeros((8, D), np.float32)
    cs[:4] = inp["c"]
    cs[4] = inp["c_ctx"]
    csT = np.ascontiguousarray(cs.T.reshape(32, 128, 8).transpose(1, 0, 2).reshape(128, 256))
    in_maps = []
    for c in range(NCORES):
        sl = slice(c * MCOLS, (c + 1) * MCOLS)
        mw = np.stack([np.ascontiguousarray(inp["l%d_mod_w" % l][:, sl]) for l in range(2)])
        mb = np.stack([np.broadcast_to(inp["l%d_mod_b" % l][sl], (8, MCOLS)) for l in range(2)])
        in_maps.append({"csT": csT, "mw": mw, "mb": np.ascontiguousarray(mb)})
    nc = build_mod()
    res = run_bass_kernel_spmd(nc, in_maps, core_ids=list(range(NCORES)))
    mod = np.concatenate([r["mod"] for r in res.results], axis=2)
    return mod[:, :5]


NT = 2304
NTH = 1152
EPS = 1e-6


def emit_norm_proj(k, P, arena, xs, pv, ident, wl, ncb, tok_major_blocks, zT, vtm, mod_cols, n_ctx_tiles=2,
                   xs_b=None, xsum=None):
    off = 0
    xt, off = k.carve(arena, off, 4096, F32, "xt")
    xn, off = k.carve(arena, off, 4096, F32, "xn")
    hT, off = k.carve(arena, off, 32 * NTH, BF16, "hT")
    wst = []
    wbf = []
    zsb = []
    for i in range(2):
        t, off = k.carve(arena, off, 4096, F32, "wst%d" % i)
        wst.append(t)
    for i in range(2):
        t, off = k.carve(arena, off, 4096, BF16, "wbf%d" % i)
        wbf.append(t)
    for i in range(2):
        t, off = k.carve(arena, off, NTH, F32, "zsb%d" % i)
        zsb.append(t)
    sm, off = k.carve(arena, off, 8, F32, "sm")
    AB, off = k.carve(arena, off, 128, F32, "AB")
    c0 = mod_cols
    for i, sc_off in enumerate((c0 + 32, c0 + 96)):
        k.stt(AB[:, i * 32:(i + 1) * 32], pv[:, sc_off:sc_off + 32], 1.0, pv[:, c0:c0 + 32], ALU.add, ALU.mult,
              [pv], [AB])
    sh_l = pv[:, c0 + 64:c0 + 96]
    sh_c = pv[:, c0 + 128:c0 + 160]
    wcount = 0
    for th in range(2):
        for tl in range(9):
            ti = th * 9 + tl
            is_ctx = ti < n_ctx_tiles
            k.dma("sp", xt[:], xs[ti * 128:(ti + 1) * 128, :], writes=[xt], key=xt)
            if xs_b is not None:
                k.dma("pool", xn[:], xs_b[ti * 128:(ti + 1) * 128, :], writes=[xn], key=xn)
                k.tt("dve", xt[:], xt[:], xn[:], ALU.add, [xt, xn], [xt])
                k.dma("sp", xsum[ti * 128:(ti + 1) * 128, :], xt[:], reads=[xt], key=xt)
            k.act(xn[:], xt[:], AF.Square, [xt], [xn, sm], accum_out=sm[:, 0:1])
            k.ts("dve", sm[:, 1:2], sm[:, 0:1], 1.0 / D, EPS, ALU.mult, ALU.add, [sm], [sm])
            k.act(sm[:, 2:3], sm[:, 1:2], AF.Sqrt, [sm], [sm])
            k.op("dve", lambda be: be.reciprocal(out=sm[:, 3:4], in_=sm[:, 2:3]), [sm], [sm])
            k.ts("dve", xn[:], xt[:], sm[:, 3:4], None, ALU.mult, None, [xt, sm], [xn])
            for bnk in range(8):
                for q in range(4):
                    kt = bnk * 4 + q
                    k.tr(P[bnk][:, q * 128:(q + 1) * 128], xn[:, kt * 128:(kt + 1) * 128], ident[:, 0:128],
                         [xn, ident], [P[bnk]], inc=(q == 3))
                for q in range(4):
                    kt = bnk * 4 + q
                    a_ap = AB[:, 32 + kt:33 + kt] if is_ctx else AB[:, kt:kt + 1]
                    s_ap = sh_c[:, kt:kt + 1] if is_ctx else sh_l[:, kt:kt + 1]
                    k.act(hT[:, kt * NTH + tl * 128: kt * NTH + (tl + 1) * 128], P[bnk][:, q * 128:(q + 1) * 128],
                          AF.Identity, [P[bnk], AB, pv], [hT], ss=True, scale=a_ap, bias=s_ap)
        for j in range(ncb):
            ws = wst[wcount % 2]
            wb = wbf[wcount % 2]
            k.dma("sp" if wcount % 2 == 0 else "pool", ws[:], wl[j], writes=[ws], key=ws)
            k.copy("dve", wb[:], ws[:], [ws], [wb])
            if j not in tok_major_blocks:
                banks = [P[(wcount % 2) * 3 + tc] for tc in range(3)]
                for kt in range(32):
                    for tc in range(3):
                        k.mm(banks[tc][:, 0:384], wb[:, kt * 128:(kt + 1) * 128],
                             hT[:, kt * NTH + tc * 384: kt * NTH + (tc + 1) * 384],
                             kt == 0, kt == 31, [wb, hT], [banks[tc]], inc=(kt == 31))
                zs = zsb[wcount % 2]
                for tc in range(3):
                    k.copy("act", zs[:, tc * 384:(tc + 1) * 384], banks[tc][:, 0:384], [banks[tc]], [zs])
                k.dma("sp", zT[j * 128:(j + 1) * 128, th * NTH:(th + 1) * NTH], zs[:], reads=[zs], key=zs)
            else:
                idx = tok_major_blocks[j]
                zs = zsb[wcount % 2]
                for tb in range(9):
                    bank = P[6 + tb % 2]
                    col = 0
                    for kt in range(32):
                        k.mm(bank[:, col:col + 128], hT[:, kt * NTH + tb * 128: kt * NTH + (tb + 1) * 128],
                             wb[:, kt * 128:(kt + 1) * 128], kt == 0, kt == 31, [wb, hT], [bank], inc=(kt == 31))
                    k.copy("act", zs[:, tb * 128:(tb + 1) * 128], bank[:, col:col + 128], [bank], [zs])
                nv = vtm.t.shape[1]
                dst = vtm[th * NTH:(th + 1) * NTH, idx * 128:(idx + 1) * 128].rearrange("(tb p) c -> p tb c", p=128)
                src = zs[:, :].rearrange("p (tb c) -> p tb c", c=128)
                k.dma("sp", dst, src, reads=[zs], key=zs)
            wcount += 1


def a0_weight_cols(hf):
    cols = []
    for g in range(6):
        cols.append(np.arange(g * 2048 + hf * 1024, g * 2048 + (hf + 1) * 1024))
    return np.concatenate(cols)


def layout_w_blocks(w, cols):
    ws = w[:, cols]
    ncb = ws.shape[1] // 128
    return np.ascontiguousarray(ws.reshape(32, 128, ncb, 128).transpose(2, 1, 0, 3).reshape(ncb, 128, 4096))


def feat_major(v):
    return np.ascontiguousarray(v.reshape(32, 128).T)


NCTX = 256
SEGS = ((0, NCTX), (NCTX, NT))
CHUNKS = ((0, 512), (512, 1024), (1024, 1536), (1536, 2048), (2048, 2304))


def emit_gelu_tanh(k, out, y, t1, t2, hsum, R, W):
    k.tt("dve", t1, y, y, ALU.mult, R, W)
    k.ts("dve", t1, t1, 0.044715, 1.0, ALU.mult, ALU.add, R, W)
    k.tt("dve", t1, t1, y, ALU.mult, R, W)
    k.act(t2, t1, AF.Tanh, R, W, scale=0.7978845608028654)
    k.stt(t2, t2, 1.0, y, ALU.add, ALU.mult, R, W)
    k.stt(out, hsum, 0.5, t2, ALU.mult, ALU.mult, R, W)


def emit_lru(k, P, arena, off0, zT, yT, pl, wa, wx, cl, zrow0=0):
    off = off0
    bufs = {}
    for nm in ("xa", "ya", "u", "rb", "ib", "tb", "bb", "hf", "hr"):
        bufs[nm], off = k.carve(arena, off, NT, F32, "lru_" + nm)
    xa, ya, u, rb, ib, tb, bb, hf_, hr_ = [bufs[n] for n in ("xa", "ya", "u", "rb", "ib", "tb", "bb", "hf", "hr")]
    CW, CB, BA, BX, LAM = 0, 32, 40, 56, 72
    k.act(cl[:, 0:16], pl[:, LAM:LAM + 16], AF.Exp, [pl], [cl], scale=-1.0)
    k.act(cl[:, 0:16], cl[:, 0:16], AF.Ln, [cl], [cl], bias=1.0)
    k.ts("dve", cl[:, 0:16], cl[:, 0:16], -8.0, None, ALU.mult, None, [cl], [cl])
    for i in range(8):
        k.dma("sp", xa[:], zT[zrow0 + i * 128:zrow0 + (i + 1) * 128, :], writes=[xa], key=xa)
        k.dma("pool", ya[:], zT[zrow0 + 1024 + i * 128:zrow0 + 1024 + (i + 1) * 128, :], writes=[ya], key=ya)
        k.ts("dve", u[:], xa[:], pl[:, CW + i * 4 + 2:CW + i * 4 + 3], pl[:, CB + i:CB + i + 1], ALU.mult, ALU.add,
             [xa, pl], [u])
        for (s, e) in SEGS:
            for j, sh in ((0, -2), (1, -1), (3, 1)):
                wj = pl[:, CW + i * 4 + j:CW + i * 4 + j + 1]
                if sh < 0:
                    o_ap, i_ap = u[:, s - sh:e], xa[:, s:e + sh]
                else:
                    o_ap, i_ap = u[:, s:e - sh], xa[:, s + sh:e]
                k.stt(o_ap, i_ap, wj, o_ap, ALU.mult, ALU.add, [xa, pl, u], [u])
        for d in range(2):
            wsl = slice((d * 8 + i) * 128, (d * 8 + i + 1) * 128)
            for ci, (c0, c1) in enumerate(CHUNKS):
                n = c1 - c0
                pa = P[(2 * ci) % 8]
                px = P[(2 * ci + 1) % 8]
                k.mm(pa[:, 0:n], wa[:, wsl], u[:, c0:c1], True, True, [wa, u], [pa])
                k.mm(px[:, 0:n], wx[:, wsl], u[:, c0:c1], True, True, [wx, u], [px])
                k.act(rb[:, c0:c1], pa[:, 0:n], AF.Sigmoid, [pa, pl], [rb], ss=True,
                      bias=pl[:, BA + d * 8 + i:BA + d * 8 + i + 1])
                k.act(ib[:, c0:c1], px[:, 0:n], AF.Sigmoid, [px, pl], [ib], ss=True,
                      bias=pl[:, BX + d * 8 + i:BX + d * 8 + i + 1])
            k.act(rb[:], rb[:], AF.Exp, [rb, cl], [rb], scale=cl[:, d * 8 + i:d * 8 + i + 1])
            k.tt("dve", tb[:], rb[:], rb[:], ALU.mult, [rb], [tb])
            k.act(tb[:], tb[:], AF.Sqrt, [tb], [tb], scale=-1.0, bias=1.0)
            k.tt("dve", ib[:], ib[:], u[:], ALU.mult, [ib, u], [ib])
            k.tt("dve", bb[:], tb[:], ib[:], ALU.mult, [tb, ib], [bb])
            h = hf_ if d == 0 else hr_
            if d == 0:
                k.op("dve", lambda be, h=h: be.tensor_tensor_scan(
                    out=h[:, 0:NCTX], data0=rb[:, 0:NCTX], data1=bb[:, 0:NCTX], initial=0.0,
                    op0=ALU.mult, op1=ALU.add), [rb, bb], [h])
                k.op("dve", lambda be, h=h: be.tensor_tensor_scan(
                    out=h[:, NCTX:NT], data0=rb[:, NCTX:NT], data1=bb[:, NCTX:NT], initial=h[:, NCTX - 1:NCTX],
                    op0=ALU.mult, op1=ALU.add), [rb, bb, h], [h])
            else:
                k.op("dve", lambda be, h=h: be.tensor_tensor_scan(
                    out=h[:, NCTX - 1::-1], data0=rb[:, NCTX - 1::-1], data1=bb[:, NCTX - 1::-1], initial=0.0,
                    op0=ALU.mult, op1=ALU.add), [rb, bb], [h])
                k.op("dve", lambda be, h=h: be.tensor_tensor_scan(
                    out=h[:, NT - 1:NCTX - 1:-1], data0=rb[:, NT - 1:NCTX - 1:-1], data1=bb[:, NT - 1:NCTX - 1:-1],
                    initial=h[:, 0:1], op0=ALU.mult, op1=ALU.add), [rb, bb, h], [h])
        k.tt("dve", hf_[:], hf_[:], hr_[:], ALU.add, [hf_, hr_], [hf_])
        emit_gelu_tanh(k, yT[:, i * NT:(i + 1) * NT], ya[:], tb[:], bb[:], hf_[:], [ya, tb, bb, hf_], [tb, bb, yT])


def rope_tables():
    t = np.arange(2048)
    row = (t // 64).astype(np.float32)
    col = (t % 64).astype(np.float32)
    n_freq = 32
    inv = (np.float32(10000.0) ** (-np.arange(n_freq, dtype=np.float32) / np.float32(n_freq))).astype(np.float32)
    ang = np.concatenate([row[:, None] * inv, col[:, None] * inv], axis=-1).astype(np.float32)
    cos = np.cos(ang).astype(np.float32)
    sin = np.sin(ang).astype(np.float32)
    C = np.repeat(cos.T, 2, axis=0)
    S = np.repeat(sin.T, 2, axis=0)
    Rm = np.zeros((128, 128), np.float32)
    for i in range(64):
        Rm[2 * i + 1, 2 * i] = -1.0
        Rm[2 * i, 2 * i + 1] = 1.0
    return np.ascontiguousarray(C), np.ascontiguousarray(S), Rm


def ret_consts():
    _, _, Rm = rope_tables()
    j = np.arange(128)[:, None]
    i = np.arange(128)[None, :]
    E0 = (i - j).astype(np.float32)
    Mge = (i >= j).astype(np.float32)
    Mlt = (i < j).astype(np.float32)
    return np.ascontiguousarray(np.concatenate(
        [np.eye(128, dtype=np.float32), Rm, np.ones((128, 128), np.float32), E0, Mge, Mlt], axis=1))


CST_ID, CST_RM, CST_ONES, CST_E0, CST_MGE, CST_MLT = [slice(i * 128, (i + 1) * 128) for i in range(6)]


def ret_log_decays(gh):
    e_f = 5.0 + 7.0 * gh / 15.0
    e_b = 5.0 + 7.0 * (15 - gh) / 15.0
    return float(np.log1p(-np.exp2(-e_f))), float(np.log1p(-np.exp2(-e_b)))


DEC_W = 68


def ret_dec_table(hf):
    lsc = float(np.log(128.0 ** -0.5))
    t = np.zeros((8, DEC_W), np.float64)
    for hh in range(8):
        lg_f, lg_b = ret_log_decays(hf * 8 + hh)
        t[hh, 0] = lg_f
        t[hh, 1] = -lg_b
        t[hh, 2] = lsc
        for dl in range(-15, 16):
            t[hh, 3 + dl + 15] = (lg_f * 128 * dl + lsc) if dl >= 1 else (-lg_b * 128 * dl + lsc)
        for dp in range(-1, 16):
            t[hh, 34 + dp + 1] = lg_f * (256 + 128 * dp) + lsc
            t[hh, 51 + dp + 1] = lg_b * (2048 - 128 * dp) + lsc
    return np.ascontiguousarray(np.broadcast_to(t.reshape(1, -1).astype(np.float32), (128, 8 * DEC_W)))


def emit_skewed(steps, sk=2):
    n = len(steps)
    for j in range(n + sk):
        if j < n:
            steps[j]["S"]()
            steps[j]["post"]()
        i = j - sk
        if i >= 0:
            steps[i]["PV"]()
            if steps[i].get("after") is not None:
                steps[i]["after"]()


def emit_rope(k, P, dst_bf, src, Ct, St, cst, tA, tB, banks=(6, 7)):
    k.copy("act", dst_bf[:, 0:NCTX], src[:, 0:NCTX], [src], [dst_bf])
    for c in range(4):
        cs = slice(NCTX + c * 512, NCTX + (c + 1) * 512)
        ts_ = slice(c * 512, (c + 1) * 512)
        pr = P[banks[c % 2]]
        k.mm(pr[:, :], cst[:, CST_RM], src[:, cs], True, True, [cst, src], [pr])
        k.tt("dve", tA[:, ts_], src[:, cs], Ct[:, ts_], ALU.mult, [src, Ct], [tA])
        k.tt("dve", tB[:, ts_], pr[:, :], St[:, ts_], ALU.mult, [pr, St], [tB])
        k.tt("dve", dst_bf[:, cs], tA[:, ts_], tB[:, ts_], ALU.add, [tA, tB], [dst_bf])


def emit_ret(k, P, arena, off0, zT, vtm, yT, cst, dec, q_row0, k_row0, g_row0, CS_dram, vcol0=0):
    off = off0
    qT, off = k.carve(arena, off, NT, F32, "qT")
    kT, off = k.carve(arena, off, NT, F32, "kT")
    gT = kT
    tA, off = k.carve(arena, off, NT, F32, "tA")
    tB, off = k.carve(arena, off, NT, F32, "tB")
    vst = tB
    Qb, off = k.carve(arena, off, NT, BF16, "Qb")
    Kb, off = k.carve(arena, off, NT, BF16, "Kb")
    Vb, off = k.carve(arena, off, NT, BF16, "Vb")
    Dt, off = k.carve(arena, off, 48 * 128, BF16, "Dt")
    Ct, off = k.carve(arena, off, 2048, F32, "Ct")
    St, off = k.carve(arena, off, 2048, F32, "St")
    scs = []
    for i in range(4):
        t, off = k.carve(arena, off, 512, BF16, "sc%d" % i)
        scs.append(t)
    psb = [P[0], P[1], P[4], P[5]]
    dsc, off = k.carve(arena, off, 256, F32, "dsc")
    k.dma("sp", Ct[:], CS_dram[0], writes=[Ct], key=Ct)
    k.dma("pool", St[:], CS_dram[1], writes=[St], key=St)
    E0 = cst[:, CST_E0]
    pair = 0
    for hh in range(8):
        k.dma("sp", qT[:], zT[q_row0 + hh * 128:q_row0 + (hh + 1) * 128, :], writes=[qT], key=qT)
        k.dma("pool", kT[:], zT[k_row0 + hh * 128:k_row0 + (hh + 1) * 128, :], writes=[kT], key=kT)
        k.dma("pool", vst[:].rearrange("p (kb e) -> p kb e", e=128),
              vtm[:, vcol0 + hh * 128:vcol0 + (hh + 1) * 128].rearrange("(kb p) e -> p kb e", p=128),
              writes=[vst], key=vst)
        k.copy("act", Vb[:], vst[:], [vst], [Vb])
        dc = hh * DEC_W
        sf = dec[:, dc + 0:dc + 1]
        sb = dec[:, dc + 1:dc + 2]
        b0 = dec[:, dc + 2:dc + 3]
        for dl in range(-15, 16):
            o_ap = Dt[:, (dl + 15) * 128:(dl + 16) * 128]
            bi = dec[:, dc + 3 + dl + 15:dc + 4 + dl + 15]
            if dl >= 1:
                k.act(o_ap, E0, AF.Exp, [cst, dec], [Dt], ss=True, scale=sf, bias=bi)
            elif dl <= -1:
                k.act(o_ap, E0, AF.Exp, [cst, dec], [Dt], ss=True, scale=sb, bias=bi)
            else:
                k.act(dsc[:, 0:128], E0, AF.Exp, [cst, dec], [dsc], scale=sf, bias=b0)
                k.act(dsc[:, 128:256], E0, AF.Exp, [cst, dec], [dsc], scale=sb, bias=b0)
                k.tt("dve", dsc[:, 0:128], dsc[:, 0:128], cst[:, CST_MGE], ALU.mult, [dsc, cst], [dsc])
                k.tt("dve", dsc[:, 128:256], dsc[:, 128:256], cst[:, CST_MLT], ALU.mult, [dsc, cst], [dsc])
                k.tt("dve", o_ap, dsc[:, 0:128], dsc[:, 128:256], ALU.add, [dsc], [Dt])
        for dp in range(-1, 16):
            o_ap = Dt[:, (31 + dp + 1) * 128:(31 + dp + 2) * 128]
            bf_ = dec[:, dc + 34 + dp + 1:dc + 35 + dp + 1]
            bb_ = dec[:, dc + 51 + dp + 1:dc + 52 + dp + 1]
            k.act(dsc[:, 0:128], E0, AF.Exp, [cst, dec], [dsc], scale=sf, bias=bf_)
            k.act(dsc[:, 128:256], E0, AF.Exp, [cst, dec], [dsc], scale=sb, bias=bb_)
            k.tt("dve", o_ap, dsc[:, 0:128], dsc[:, 128:256], ALU.add, [dsc], [Dt])
        emit_rope(k, P, Qb, qT, Ct, St, cst, tA, tB)
        emit_rope(k, P, Kb, kT, Ct, St, cst, tA, tB)
        oT = tA
        groups = [(0, 256, [(kb, 15 - kb) for kb in range(2)])]
        for g in range(4):
            lst = [(kb, 31 + (4 * g - kb) + 1) for kb in range(2)]
            lst += [(2 + kbl, 4 * g - kbl + 15) for kbl in range(16)]
            groups.append((NCTX + g * 512, 512, lst))
        steps = []
        for gi, (c0, n, lst) in enumerate(groups):
            po = P[2 + gi % 2]
            for li, (kb, tix) in enumerate(lst):
                ps = psb[pair % 4]
                sc = scs[pair % 4]
                pair += 1
                st = {}
                st["S"] = (lambda ps=ps, kb=kb, c0=c0, n=n: k.mm(
                    ps[:, 0:n], Kb[:, kb * 128:(kb + 1) * 128], Qb[:, c0:c0 + n], True, True, [Kb, Qb], [ps]))
                st["post"] = (lambda ps=ps, sc=sc, tix=tix, n=n: k.tt(
                    "dve", sc[:, 0:n], ps[:, 0:n], Dt[:, tix * 128:tix * 128 + n], ALU.mult, [ps, Dt], [sc]))
                st["PV"] = (lambda po=po, sc=sc, kb=kb, n=n, first=(li == 0), last=(li == len(lst) - 1): k.mm(
                    po[:, 0:n], Vb[:, kb * 128:(kb + 1) * 128], sc[:, 0:n], first, last, [Vb, sc], [po], inc=True))
                if li == len(lst) - 1:
                    st["after"] = (lambda po=po, c0=c0, n=n: k.copy("act", oT[:, c0:c0 + n], po[:, 0:n], [po], [oT]))
                steps.append(st)
        emit_skewed(steps)
        k.tt("dve", tB[:], oT[:], oT[:], ALU.mult, [oT], [tB])
        for ci, (c0, c1) in enumerate(CHUNKS):
            n = c1 - c0
            pn = P[6 + ci % 2]
            k.mm(pn[:, 0:n], cst[:, CST_ONES], tB[:, c0:c1], True, True, [cst, tB], [pn])
            k.act(qT[:, c0:c1], pn[:, 0:n], AF.Sqrt, [pn], [qT], ss=True, scale=1.0 / 128.0, bias=EPS)
        k.op("dve", lambda be: be.reciprocal(out=qT[:], in_=qT[:]), [qT], [qT])
        k.tt("dve", tB[:], oT[:], qT[:], ALU.mult, [oT, qT], [tB])
        k.dma("sp", gT[:], zT[g_row0 + hh * 128:g_row0 + (hh + 1) * 128, :], writes=[gT], key=gT)
        k.act(gT[:], gT[:], AF.Silu, [gT], [gT])
        k.tt("dve", yT[:, (8 + hh) * NT:(9 + hh) * NT], tB[:], gT[:], ALU.mult, [tB, gT], [yT])
    return off


def emit_wout(k, P, arena, off0, yT, wol, xs, grep, rm, so, n_tb=18, n_ctx_tb=2, nt=NT, x_row0=0):
    off = off0
    wst, wbf, xt, ot, gt = [], [], [], [], []
    for i in range(2):
        t, off = k.carve(arena, off, 4096, F32, "wo_st%d" % i)
        wst.append(t)
    for i in range(2):
        t, off = k.carve(arena, off, 8192, BF16, "wo_bf%d" % i)
        wbf.append(t)
    for i in range(2):
        t, off = k.carve(arena, off, 512, F32, "wo_x%d" % i)
        xt.append(t)
        t, off = k.carve(arena, off, 512, F32, "wo_o%d" % i)
        ot.append(t)
        t, off = k.carve(arena, off, 1024, F32, "wo_g%d" % i)
        gt.append(t)
    cnt = 0
    for n in range(8):
        wb = wbf[n % 2]
        g = gt[n % 2]
        k.dma("pool", g[:, 0:512], grep[0, :, n * 512:(n + 1) * 512], writes=[g], key=g)
        k.dma("pool", g[:, 512:1024], grep[1, :, n * 512:(n + 1) * 512], writes=[g], key=g)
        for hk in range(2):
            ws = wst[hk]
            k.dma("sp", ws[:], wol[n, :, hk * 4096:(hk + 1) * 4096], writes=[ws], key=ws)
            k.copy("act" if hk == 0 else "dve", wb[:, hk * 4096:(hk + 1) * 4096], ws[:], [ws], [wb])
        for tb in range(n_tb):
            pb = P[4 + tb % 4]
            for kc in range(16):
                k.mm(pb[:, :], yT[:, kc * nt + tb * 128: kc * nt + (tb + 1) * 128], wb[:, kc * 512:(kc + 1) * 512],
                     kc == 0, kc == 15, [yT, wb], [pb], inc=(kc == 15))
            x_ = xt[cnt % 2]
            o_ = ot[cnt % 2]
            cnt += 1
            k.dma("sp", x_[:], xs[x_row0 + tb * 128:x_row0 + (tb + 1) * 128, n * 512:(n + 1) * 512], writes=[x_], key=x_)
            gs = g[:, 512:1024] if tb < n_ctx_tb else g[:, 0:512]
            k.tt("dve", o_[:], pb[:, :], gs, ALU.mult, [pb, g], [o_])
            k.stt(o_[:], x_[:], rm[:, 0:1], o_[:], ALU.mult, ALU.add, [x_, rm, o_], [o_])
            k.dma("sp", so[tb * 128:(tb + 1) * 128, n * 512:(n + 1) * 512], o_[:], reads=[o_], key=o_, is_output=True)


def build_a0(scratch_kind="Internal"):
    k = K()
    xs = k.dram("xs", [NT, D], F32, "ExternalInput")
    pvd = k.dram("pv", [128, 160], F32, "ExternalInput")
    cstd = k.dram("cst", [128, 768], F32, "ExternalInput")
    wl = k.dram("wl", [48, 128, 4096], F32, "ExternalInput")
    pld = k.dram("pl", [128, 88], F32, "ExternalInput")
    wad = k.dram("wa", [128, 2048], F32, "ExternalInput")
    wxd = k.dram("wx", [128, 2048], F32, "ExternalInput")
    CS = k.dram("CS", [2, 128, 2048], F32, "ExternalInput")
    wol = k.dram("wol", [8, 128, 8192], F32, "ExternalInput")
    grep = k.dram("grep", [2, 128, D], F32, "ExternalInput")
    rmd = k.dram("rm", [128, 1], F32, "ExternalInput")
    dcd = k.dram("dec", [128, 8 * DEC_W], F32, "ExternalInput")
    so = k.dram("so", [NT, D], F32, "ExternalOutput")
    zT = k.dram("zT", [48 * 128, NT], F32, scratch_kind)
    vtm = k.dram("vtm", [NT, 1024], F32, scratch_kind)
    arena = k.sbuf("arena", [128, 43500], F32)
    pv = k.sbuf("pvs", [128, 160], F32)
    cst = k.sbuf("csts", [128, 768], F32)
    pl = k.sbuf("pls", [128, 88], F32)
    wa = k.sbuf("was", [128, 2048], F32)
    wx = k.sbuf("wxs", [128, 2048], F32)
    cl = k.sbuf("cl", [128, 16], F32)
    rm = k.sbuf("rms", [128, 1], F32)
    dec = k.sbuf("decs", [128, 8 * DEC_W], F32)
    P = [k.psum("P%d" % i, [128, 512]) for i in range(8)]
    for a, b_ in ((pv, pvd), (cst, cstd), (pl, pld), (wa, wad), (wx, wxd), (rm, rmd), (dec, dcd)):
        k.dma("sp", a[:], b_[:], writes=[a], key=a)
    tm = {32 + i: i for i in range(8)}
    emit_norm_proj(k, P, arena, xs, pv, cst, wl, 48, tm, zT, vtm, 0)
    k.barrier()
    yT, off = k.carve(arena, 0, 16 * NT, BF16, "yT")
    emit_lru(k, P, arena, off, zT, yT, pl, wa, wx, cl)
    k.barrier()
    emit_ret(k, P, arena, off, zT, vtm, yT, cst, dec, 2048, 3072, 5120, CS)
    k.barrier()
    emit_wout(k, P, arena, off, yT, wol, xs, grep, rm, so)
    return k.finish()


def lru_params(inp, hf):
    sl = slice(hf * 1024, (hf + 1) * 1024)
    cw = inp["l0_conv_w"][:, sl].reshape(4, 8, 128).transpose(2, 1, 0).reshape(128, 32)
    cb = inp["l0_conv_b"][sl].reshape(8, 128).T

    def dv(a):
        return a[:, sl].reshape(2, 8, 128).transpose(2, 0, 1).reshape(128, 16)
    pl = np.concatenate([cw, cb, dv(inp["l0_lru_ra_b"]), dv(inp["l0_lru_ix_b"]), dv(inp["l0_lru_lambda"])], axis=1)

    def wm(a):
        return np.ascontiguousarray(a[:, hf * 8:(hf + 1) * 8].transpose(2, 0, 1, 3).reshape(128, 2048))
    return np.ascontiguousarray(pl.astype(np.float32)), wm(inp["l0_lru_ra_w"]), wm(inp["l0_lru_ix_w"])


def layout_wout(w_out, rows):
    ws = w_out[rows]
    return np.ascontiguousarray(ws.reshape(16, 128, 8, 512).transpose(2, 1, 0, 3).reshape(8, 128, 8192))


def a0_inputs(inp, mod0, b, hf, consts):
    xs = np.concatenate([inp["ctx"][b], inp["x"][b]], axis=0)
    m = mod0
    pv = np.concatenate([feat_major(inp["l0_norm1"]), feat_major(m[b, D:2 * D]), feat_major(m[b, 0:D]),
                         feat_major(m[4, D:2 * D]), feat_major(m[4, 0:D])], axis=1)
    pl, wa, wx = lru_params(inp, hf)
    rows = np.concatenate([np.arange(hf * 1024, (hf + 1) * 1024), 2048 + np.arange(hf * 1024, (hf + 1) * 1024)])
    grep = np.stack([np.broadcast_to(m[b, 2 * D:3 * D], (128, D)), np.broadcast_to(m[4, 2 * D:3 * D], (128, D))])
    return {"xs": xs, "pv": np.ascontiguousarray(pv), "cst": consts["cst"],
            "wl": layout_w_blocks(inp["l0_w_in"], a0_weight_cols(hf)), "pl": pl, "wa": wa, "wx": wx,
            "CS": consts["CS"], "wol": layout_wout(inp["l0_w_out"], rows), "grep": np.ascontiguousarray(grep),
            "rm": np.full((128, 1), 1.0 if hf == 0 else 0.0, np.float32), "dec": ret_dec_table(hf)}


def make_consts():
    C, S, _ = rope_tables()
    return {"cst": ret_consts(), "CS": np.ascontiguousarray(np.stack([C, S]))}


def build_f(n_ctx):
    ntf = n_ctx + 2048
    ntile = ntf // 128
    n_ctx_tiles = n_ctx // 128
    nch = 3 if n_ctx else 2
    NS = 256 + (32 if n_ctx else 0)
    ch_rows = [128, 128, 32][:nch]
    ch_col = [0, 128, 256][:nch]
    k = K()
    s01 = k.dram("s01", [2, ntf, D], F32, "ExternalInput")
    pvd = k.dram("pv", [128, 160], F32, "ExternalInput")
    rtd = k.dram("rt", [128, 512], F32, "ExternalInput")
    cstd = k.dram("cst", [128, 768], F32, "ExternalInput")
    w13l = k.dram("w13l", [8, 2, 8, 128, 4096], F32, "ExternalInput")
    w2l = k.dram("w2l", [8, 8, 128, 4096], F32, "ExternalInput")
    grep = k.dram("grep", [2, 128, D], F32, "ExternalInput")
    rmd = k.dram("rm", [128, 1], F32, "ExternalInput")
    so2 = k.dram("so", [ntf * 2, 2048], F32, "ExternalOutput")
    so = so2.t.rearrange("(t h) c -> t (h c)", h=2)
    xns = k.dram("xns", [ntf, D], F32, "Internal")
    arena = k.sbuf("arena", [128, 41000], F32)
    pv = k.sbuf("pvs", [128, 160], F32)
    rt = k.sbuf("rts", [128, 512], F32)
    cst = k.sbuf("csts", [128, 768], F32)
    rm = k.sbuf("rms", [128, 1], F32)
    AB = k.sbuf("AB", [128, 64], F32)
    affT = k.sbuf("affT", [16, ntf], F32)
    sm = k.sbuf("sm", [128, 8], F32)
    P = [k.psum("P%d" % i, [128, 512]) for i in range(8)]
    for a, b_ in ((pv, pvd), (rt, rtd), (cst, cstd), (rm, rmd)):
        k.dma("sp", a[:], b_[:], writes=[a], key=a)
    ident = cst[:, CST_ID]
    for i, sc_off in enumerate((32, 96)):
        k.stt(AB[:, i * 32:(i + 1) * 32], pv[:, sc_off:sc_off + 32], 1.0, pv[:, 0:32], ALU.add, ALU.mult, [pv], [AB])
    sh_l = pv[:, 64:96]
    sh_c = pv[:, 128:160]

    off = 0
    s0t, off = k.carve(arena, off, 4096, F32, "s0t")
    s1t, off = k.carve(arena, off, 4096, F32, "s1t")
    x1t, off = k.carve(arena, off, 4096, F32, "x1t")
    ott, off = k.carve(arena, off, 4096, F32, "ott")
    xnt, off = k.carve(arena, off, 4096, F32, "xnt")
    h2T, off = k.carve(arena, off, 4096, F32, "h2T")
    ex, off = k.carve(arena, off, 32, F32, "ex")
    for ti in range(ntile):
        is_ctx = ti < n_ctx_tiles
        rows = slice(ti * 128, (ti + 1) * 128)
        k.dma("sp", s0t[:], s01[0, rows, :], writes=[s0t], key=s0t)
        k.dma("pool", s1t[:], s01[1, rows, :], writes=[s1t], key=s1t)
        k.tt("dve", x1t[:], s0t[:], s1t[:], ALU.add, [s0t, s1t], [x1t])
        k.ts("dve", ott[:], x1t[:], rm[:, 0:1], None, ALU.mult, None, [x1t, rm], [ott])
        k.dma("sp", so[rows, :], ott[:], reads=[ott], key=ott, is_output=True)
        k.act(xnt[:], x1t[:], AF.Square, [x1t], [xnt, sm], accum_out=sm[:, 0:1])
        k.ts("dve", sm[:, 1:2], sm[:, 0:1], 1.0 / D, EPS, ALU.mult, ALU.add, [sm], [sm])
        k.act(sm[:, 2:3], sm[:, 1:2], AF.Sqrt, [sm], [sm])
        k.op("dve", lambda be: be.reciprocal(out=sm[:, 3:4], in_=sm[:, 2:3]), [sm], [sm])
        k.ts("dve", xnt[:], x1t[:], sm[:, 3:4], None, ALU.mult, None, [x1t, sm], [xnt])
        k.dma("sp", xns[rows, :], xnt[:], reads=[xnt], key=xnt)
        for bnk in range(8):
            for q in range(4):
                kt = bnk * 4 + q
                k.tr(P[bnk][:, q * 128:(q + 1) * 128], xnt[:, kt * 128:(kt + 1) * 128], ident,
                     [xnt, cst], [P[bnk]], inc=(q == 3))
            for q in range(4):
                kt = bnk * 4 + q
                a_ap = AB[:, 32 + kt:33 + kt] if is_ctx else AB[:, kt:kt + 1]
                s_ap = sh_c[:, kt:kt + 1] if is_ctx else sh_l[:, kt:kt + 1]
                k.act(h2T[:, kt * 128:(kt + 1) * 128], P[bnk][:, q * 128:(q + 1) * 128], AF.Identity,
                      [P[bnk], AB, pv], [h2T], ss=True, scale=a_ap, bias=s_ap)
        PL = P[0]
        for kt in range(32):
            k.mm(PL[:, 0:16], h2T[:, kt * 128:(kt + 1) * 128], rt[:, kt * 16:(kt + 1) * 16], kt == 0, kt == 31,
                 [h2T, rt], [PL], inc=(kt == 31))
        k.op("dve", lambda be: be.tensor_reduce(out=sm[:, 4:5], in_=PL[:, 0:16], axis=AX.X, op=ALU.max), [PL], [sm])
        k.ts("dve", sm[:, 5:6], sm[:, 4:5], -1.0, None, ALU.mult, None, [sm], [sm])
        k.act(ex[:, 0:16], PL[:, 0:16], AF.Exp, [PL, sm], [ex, sm], bias=sm[:, 5:6], accum_out=sm[:, 6:7])
        k.op("dve", lambda be: be.reciprocal(out=sm[:, 7:8], in_=sm[:, 6:7]), [sm], [sm])
        k.ts("dve", ex[:, 16:32], ex[:, 0:16], sm[:, 7:8], None, ALU.mult, None, [ex, sm], [ex])
        k.tr(P[1][0:16, 0:128], ex[:, 16:32], ident, [ex, cst], [P[1]])
        k.copy("dve", affT[:, ti * 128:(ti + 1) * 128], P[1][0:16, 0:128], [P[1]], [affT])
    k.barrier()

    off = 0
    wk_l, off = k.carve(arena, off, 2048, F32, "wk_l")
    mx, off = k.carve(arena, off, NS, F32, "mx")
    ix, off = k.carve(arena, off, NS, U32, "ix")
    ixf, off = k.carve(arena, off, NS, F32, "ixf")
    gateT, off = k.carve(arena, off, 8 * nch, F32, "gateT")
    idxf, off = k.carve(arena, off, 8 * nch * 3, F32, "idxf")
    idxu, off = k.carve(arena, off, 8 * nch * 3, U32, "idxu")
    segs = [(n_ctx, 2048, 256, 0, wk_l)]
    if n_ctx:
        wk_c, off = k.carve(arena, off, 256, F32, "wk_c")
        segs.append((0, 256, 32, 256, wk_c))
    for (t0, n, cap, m0, wk) in segs:
        k.copy("dve", wk[0:8, 0:n], affT[0:8, t0:t0 + n], [affT], [wk])
        for it in range(cap // 8):
            ms = slice(m0 + it * 8, m0 + (it + 1) * 8)
            k.op("dve", lambda be, ms=ms, wk=wk, n=n: be.max(out=mx[0:8, ms], in_=wk[0:8, 0:n]), [wk], [mx])
            k.op("dve", lambda be, ms=ms, wk=wk, n=n: be.max_index(
                out=ix[0:8, ms], in_max=mx[0:8, ms], in_values=wk[0:8, 0:n]), [wk, mx], [ix])
            k.op("dve", lambda be, ms=ms, wk=wk, n=n: be.match_replace(
                out=wk[0:8, 0:n], in_to_replace=mx[0:8, ms], in_values=wk[0:8, 0:n], imm_value=-1.0), [wk, mx], [wk])
        k.copy("dve", ixf[0:8, m0:m0 + cap], ix[0:8, m0:m0 + cap], [ix], [ixf])
        if t0:
            k.ts("dve", ixf[0:8, m0:m0 + cap], ixf[0:8, m0:m0 + cap], float(t0), None, ALU.add, None, [ixf], [ixf])
    for c in range(nch):
        r_ = ch_rows[c]
        cs = slice(ch_col[c], ch_col[c] + r_)
        k.tr(P[2][0:r_, 0:8], mx[0:8, cs], cst[0:8, 0:8], [mx, cst], [P[2]])
        k.copy("dve", gateT[0:r_, c * 8:(c + 1) * 8], P[2][0:r_, 0:8], [P[2]], [gateT])
        k.tr(P[3][0:r_, 0:8], ixf[0:8, cs], cst[0:8, 0:8], [ixf, cst], [P[3]])
        b3 = c * 24
        k.copy("dve", idxf[0:r_, b3:b3 + 8], P[3][0:r_, 0:8], [P[3]], [idxf])
        k.ts("dve", idxf[0:r_, b3 + 8:b3 + 16], idxf[0:r_, b3:b3 + 8], 2.0, None, ALU.mult, None, [idxf], [idxf])
        k.ts("dve", idxf[0:r_, b3 + 16:b3 + 24], idxf[0:r_, b3:b3 + 8], 2.0, 1.0, ALU.mult, ALU.add, [idxf], [idxf])
        k.copy("dve", idxu[0:r_, b3:b3 + 24], idxf[0:r_, b3:b3 + 24], [idxf], [idxu])

    xeT, off = k.carve(arena, off, 32 * NS, BF16, "xeT")
    hidT, off = k.carve(arena, off, 8 * NS, BF16, "hidT")
    xg, wst, wbf, gt = [], [], [], []
    for i in range(2):
        t, off = k.carve(arena, off, 4096, F32, "xg%d" % i)
        xg.append(t)
        t, off = k.carve(arena, off, 4096, F32, "wst%d" % i)
        wst.append(t)
        t, off = k.carve(arena, off, 4096, BF16, "wbf%d" % i)
        wbf.append(t)
        t, off = k.carve(arena, off, 1024, F32, "gt%d" % i)
        gt.append(t)
    tmp, off = k.carve(arena, off, NS, F32, "tmp")
    ye = []
    for i in range(nch):
        t, off = k.carve(arena, off, 2048, F32, "ye%d" % i)
        ye.append(t)
    so_dep = Dep("so_acc")
    gcnt = 0
    wcnt = 0
    for e in range(8):
        for c in range(nch):
            r_ = ch_rows[c]
            g_ = xg[gcnt % 2]
            gcnt += 1
            b3 = c * 24
            k.idma(g_[0:r_, :], xns[:, :], in_offset=bass.IndirectOffsetOnAxis(ap=idxu[0:r_, b3 + e:b3 + e + 1], axis=0),
                   reads=[idxu], writes=[g_], key=g_)
            is_ctx = (c == 2)
            for bq in range(8):
                bank = P[bq % 4]
                for q in range(4):
                    kt = bq * 4 + q
                    k.tr(bank[:, q * 128:q * 128 + r_], g_[0:r_, kt * 128:(kt + 1) * 128], cst[0:r_, 0:r_],
                         [g_, cst], [bank], inc=(q == 3))
                for q in range(4):
                    kt = bq * 4 + q
                    a_ap = AB[:, 32 + kt:33 + kt] if is_ctx else AB[:, kt:kt + 1]
                    s_ap = sh_c[:, kt:kt + 1] if is_ctx else sh_l[:, kt:kt + 1]
                    k.act(xeT[:, kt * NS + ch_col[c]:kt * NS + ch_col[c] + r_], bank[:, q * 128:q * 128 + r_],
                          AF.Identity, [bank, AB, pv], [xeT], ss=True, scale=a_ap, bias=s_ap)
        for fb in range(8):
            pbs = []
            for which in range(2):
                ws = wst[wcnt % 2]
                wb = wbf[wcnt % 2]
                k.dma("sp" if wcnt % 2 == 0 else "pool", ws[:], w13l[e, which, fb], writes=[ws], key=ws)
                k.copy("dve" if wcnt % 2 == 0 else "pool", wb[:], ws[:], [ws], [wb])
                wcnt += 1
                pb = P[4 + which]
                for kt in range(32):
                    k.mm(pb[:, 0:NS], wb[:, kt * 128:(kt + 1) * 128], xeT[:, kt * NS:(kt + 1) * NS],
                         kt == 0, kt == 31, [wb, xeT], [pb], inc=(kt == 31))
                pbs.append(pb)
            k.act(tmp[:], pbs[0][:, 0:NS], AF.Silu, [pbs[0]], [tmp])
            k.tt("dve", hidT[:, fb * NS:(fb + 1) * NS], tmp[:], pbs[1][:, 0:NS], ALU.mult, [tmp, pbs[1]], [hidT])
        for n in range(8):
            ws = wst[wcnt % 2]
            wb = wbf[wcnt % 2]
            g = gt[n % 2]
            k.dma("sp" if wcnt % 2 == 0 else "pool", ws[:], w2l[e, n], writes=[ws], key=ws)
            k.copy("dve" if wcnt % 2 == 0 else "pool", wb[:], ws[:], [ws], [wb])
            wcnt += 1
            k.dma("sp", g[:, 0:512], grep[0, :, n * 512:(n + 1) * 512], writes=[g], key=g)
            k.dma("sp", g[:, 512:1024], grep[1, :, n * 512:(n + 1) * 512], writes=[g], key=g)
            hcol = n // 4
            for c in range(nch):
                r_ = ch_rows[c]
                pb = P[6 + (n * nch + c) % 2]
                for fb in range(8):
                    k.mm(pb[0:r_, :], hidT[:, fb * NS + ch_col[c]:fb * NS + ch_col[c] + r_],
                         wb[:, fb * 512:(fb + 1) * 512], fb == 0, fb == 7, [hidT, wb], [pb], inc=(fb == 7))
                y_ = ye[c]
                gs = g[0:r_, 512:1024] if c == 2 else g[0:r_, 0:512]
                k.stt(y_[0:r_, (n % 4) * 512:(n % 4 + 1) * 512], pb[0:r_, :], gateT[0:r_, c * 8 + e:c * 8 + e + 1], gs,
                      ALU.mult, ALU.mult, [pb, gateT, g], [y_])
            if n % 4 == 3:
                for c in range(nch):
                    r_ = ch_rows[c]
                    y_ = ye[c]
                    b3 = c * 24 + 8 + hcol * 8
                    k.idma(so2[:, :], y_[0:r_, :], out_offset=bass.IndirectOffsetOnAxis(
                        ap=idxu[0:r_, b3 + e:b3 + e + 1], axis=0),
                        reads=[y_, idxu], writes=[so_dep], key=so_dep, add=True, is_output=True)
    return k.finish()


def f_inputs(inp, li, modl, b, hf, s01, consts, n_ctx):
    p = "l%d_" % li
    m = modl
    pv = np.concatenate([feat_major(inp[p + "norm2"]), feat_major(m[b, 4 * D:5 * D]), feat_major(m[b, 3 * D:4 * D]),
                         feat_major(m[4, 4 * D:5 * D]), feat_major(m[4, 3 * D:4 * D])], axis=1)
    perm = np.concatenate([np.arange(hf * 8, hf * 8 + 8), np.arange((1 - hf) * 8, (1 - hf) * 8 + 8)])
    r = inp[p + "router"][:, perm]
    rt = np.ascontiguousarray(r.reshape(32, 128, 16).transpose(1, 0, 2).reshape(128, 512))
    es = slice(hf * 8, hf * 8 + 8)
    ar = np.arange(1024)
    w13l = np.stack([np.stack([layout_w_blocks(inp[p + "exp_w1"][e], ar), layout_w_blocks(inp[p + "exp_w3"][e], ar)])
                     for e in range(hf * 8, hf * 8 + 8)])
    w2 = inp[p + "exp_w2"][es]
    w2l = np.ascontiguousarray(w2.reshape(8, 8, 128, 8, 512).transpose(0, 3, 2, 1, 4).reshape(8, 8, 128, 4096))
    grep = np.stack([np.broadcast_to(m[b, 5 * D:6 * D], (128, D)), np.broadcast_to(m[4, 5 * D:6 * D], (128, D))])
    return {"s01": s01, "pv": np.ascontiguousarray(pv), "rt": rt, "cst": consts["cst"], "w13l": w13l, "w2l": w2l,
            "grep": np.ascontiguousarray(grep), "rm": np.full((128, 1), 1.0 if hf == 0 else 0.0, np.float32)}


NLAT = 2048
LCH = ((0, 512), (512, 1024), (1024, 1536), (1536, 2048))


def a1_weight_cols(hf):
    gq = np.arange(hf * 1024, (hf + 1) * 1024)
    nq = 2048 + np.arange(hf * 1024, (hf + 1) * 1024)
    gk = 4096 + np.arange(hf * 256, (hf + 1) * 256)
    gv = 4608 + np.arange(hf * 256, (hf + 1) * 256)
    nk = 5120 + np.arange(hf * 1024, (hf + 1) * 1024)
    nv = 7168 + np.arange(hf * 1024, (hf + 1) * 1024)
    return np.concatenate([gq, nq, gk, gv, nk, nv])


def na_tables(rpb_h):
    NEG = np.float32(-30000.0)

    def table(rho, kappa):
        t = np.full((2, 64, 2, 64), NEG, np.float32)
        for kr_l in range(2):
            kr = 2 * kappa + kr_l
            for r_l in range(2):
                r = 2 * rho + r_l
                r0 = min(max(r - 4, 0), 24)
                if not (r0 <= kr < r0 + 8):
                    continue
                c = np.arange(64)
                c0 = np.clip(c - 8, 0, 48)
                kc = np.arange(64)
                inwin = (kc[:, None] >= c0[None, :]) & (kc[:, None] < c0[None, :] + 16)
                relc = np.clip(kc[:, None] - c[None, :] + 15, 0, 30)
                vals = rpb_h[kr - r + 7][relc]
                t[kr_l, :, r_l, :] = np.where(inwin, vals, NEG)
        return t.reshape(128, 128)
    tabs = [table(8, 8 + d) for d in range(-2, 3)]
    for rho, k0 in ((0, 0), (1, 0), (14, 12), (15, 12)):
        tabs += [table(rho, k0 + i) for i in range(4)]
    return np.stack(tabs)


def na_key_list(rho):
    if rho == 0:
        return [(i, 5 + i) for i in range(4)]
    if rho == 1:
        return [(i, 9 + i) for i in range(4)]
    if rho == 14:
        return [(12 + i, 13 + i) for i in range(4)]
    if rho == 15:
        return [(12 + i, 17 + i) for i in range(4)]
    return [(rho + d, d + 2) for d in range(-2, 3)]


def emit_headnorm(k, P, dst, src, n, gain_ap, gain_dep, cst, tA, tB, banks=(6, 7)):
    k.tt("dve", tA[:, 0:n], src[:, 0:n], src[:, 0:n], ALU.mult, [src], [tA])
    c0 = 0
    i = 0
    while c0 < n:
        c1 = min(c0 + 512, n)
        pn = P[banks[i % 2]]
        i += 1
        k.mm(pn[:, 0:c1 - c0], cst[:, CST_ONES], tA[:, c0:c1], True, True, [cst, tA], [pn])
        k.act(tB[:, c0:c1], pn[:, 0:c1 - c0], AF.Sqrt, [pn], [tB], ss=True, scale=1.0 / 128.0, bias=EPS)
        c0 = c1
    k.op("dve", lambda be: be.reciprocal(out=tB[:, 0:n], in_=tB[:, 0:n]), [tB], [tB])
    k.stt(dst[:, 0:n], src[:, 0:n], gain_ap, tB[:, 0:n], ALU.mult, ALU.mult, [src, tB, gain_dep], [dst])


def emit_rope_lat(k, P, dst_bf, dcol0, src, scol0, Ct, St, cst, tA, tB, banks=(6, 7)):
    for c in range(4):
        ss_ = slice(scol0 + c * 512, scol0 + (c + 1) * 512)
        ds_ = slice(dcol0 + c * 512, dcol0 + (c + 1) * 512)
        ts_ = slice(c * 512, (c + 1) * 512)
        pr = P[banks[c % 2]]
        k.mm(pr[:, :], cst[:, CST_RM], src[:, ss_], True, True, [cst, src], [pr])
        k.tt("dve", tA[:, ts_], src[:, ss_], Ct[:, ts_], ALU.mult, [src, Ct], [tA])
        k.tt("dve", tB[:, ts_], pr[:, :], St[:, ts_], ALU.mult, [pr, St], [tB])
        k.tt("dve", dst_bf[:, ds_], tA[:, ts_], tB[:, ts_], ALU.add, [tA, tB], [dst_bf])


def emit_attn_cd(k, P, arena, off0, zT, vtm, yT, cst, gn, CS_dram, Btd, zb0=0, vidx0=0):
    off = off0
    s32, off = k.carve(arena, off, NT, F32, "s32")
    n32, off = k.carve(arena, off, NT, F32, "n32")
    tA, off = k.carve(arena, off, NT, F32, "tA")
    tB, off = k.carve(arena, off, NT, F32, "tB")
    KT, off = k.carve(arena, off, NT, BF16, "KT")
    QT, off = k.carve(arena, off, NLAT, BF16, "QT")
    Vb, off = k.carve(arena, off, NT, BF16, "Vb")
    Ct, off = k.carve(arena, off, 2048, F32, "Ct")
    St, off = k.carve(arena, off, 2048, F32, "St")
    Es = []
    for i in range(4):
        t, off = k.carve(arena, off, 512, BF16, "E%d" % i)
        Es.append(t)
    psb = [P[0], P[1], P[6], P[7]]
    rden, off = k.carve(arena, off, 512, F32, "rden")
    Bt, off = k.carve(arena, off, 21 * 128, F32, "Bt")
    Bx, off = k.carve(arena, off, 21 * 128, BF16, "Bx")
    onesb, off = k.carve(arena, off, 128, BF16, "onesb")
    k.dma("sp", Ct[:], CS_dram[0], writes=[Ct], key=Ct)
    k.dma("pool", St[:], CS_dram[1], writes=[St], key=St)
    k.copy("dve", onesb[:], cst[:, CST_ONES], [cst], [onesb])
    scale = 128.0 ** -0.5
    vst = tA
    ecnt = 0

    def load_v(idx):
        idx = idx + vidx0
        k.dma("pool", vst[:].rearrange("p (kb e) -> p kb e", e=128),
              vtm[:, idx * 128:(idx + 1) * 128].rearrange("(kb p) e -> p kb e", p=128), writes=[vst], key=vst)
        k.copy("act", Vb[:], vst[:], [vst], [Vb])

    def finish_group(po, pd, n, ycol):
        k.op("dve", lambda be: be.reciprocal(out=rden[:, 0:n], in_=pd[:, 0:n]), [pd], [rden])
        k.tt("dve", yT[:, ycol:ycol + n], po[:, 0:n], rden[:, 0:n], ALU.mult, [po, rden], [yT])

    for kvh in range(2):
        k.dma("sp", s32[:], zT[(zb0 + 16 + kvh) * 128:(zb0 + 17 + kvh) * 128, :], writes=[s32], key=s32)
        load_v(kvh)
        emit_headnorm(k, P, n32, s32, NT, gn[:, 1:2], gn, cst, tA, tB)
        k.copy("act", KT[:, 0:NCTX], n32[:, 0:NCTX], [n32], [KT])
        emit_rope_lat(k, P, KT, NCTX, n32, NCTX, Ct, St, cst, tA, tB)
        for qi in range(4):
            qh = kvh * 4 + qi
            k.dma("sp", s32[:, 0:NLAT], zT[(zb0 + qh) * 128:(zb0 + qh + 1) * 128, NCTX:NT], writes=[s32], key=s32)
            emit_headnorm(k, P, n32, s32, NLAT, gn[:, 0:1], gn, cst, tA, tB)
            emit_rope_lat(k, P, QT, 0, n32, 0, Ct, St, cst, tA, tB)
            steps = []
            for qg in range(4):
                po = P[2 + qg % 2]
                pd = P[4 + qg % 2]
                for kb in range(18):
                    ps = psb[ecnt % 4]
                    E = Es[ecnt % 4]
                    ecnt += 1
                    st = {}
                    st["S"] = (lambda ps=ps, kb=kb, qg=qg: k.mm(
                        ps[:, :], KT[:, kb * 128:(kb + 1) * 128], QT[:, qg * 512:(qg + 1) * 512], True, True,
                        [KT, QT], [ps]))
                    st["post"] = (lambda ps=ps, E=E: k.act(E[:], ps[:, :], AF.Exp, [ps], [E], scale=scale))

                    def pv(po=po, pd=pd, E=E, kb=kb):
                        k.mm(po[:, :], Vb[:, kb * 128:(kb + 1) * 128], E[:], kb == 0, kb == 17, [Vb, E], [po])
                        k.mm(pd[:, :], onesb[:], E[:], kb == 0, kb == 17, [onesb, E], [pd])
                    st["PV"] = pv
                    if kb == 17:
                        st["after"] = (lambda po=po, pd=pd, qh=qh, qg=qg: finish_group(po, pd, 512, qh * NLAT + qg * 512))
                    steps.append(st)
            emit_skewed(steps)
    for h in range(8):
        k.dma("sp", Bt[:], Btd[h], writes=[Bt], key=Bt)
        k.act(Bx[:], Bt[:], AF.Exp, [Bt], [Bx])
        k.dma("sp", s32[:], zT[(zb0 + 20 + h) * 128:(zb0 + 21 + h) * 128, :], writes=[s32], key=s32)
        load_v(2 + h)
        emit_headnorm(k, P, KT, s32, NT, gn[:, 3:4], gn, cst, tA, tB)
        k.dma("sp", s32[:, 0:NLAT], zT[(zb0 + 8 + h) * 128:(zb0 + 9 + h) * 128, NCTX:NT], writes=[s32], key=s32)
        emit_headnorm(k, P, QT, s32, NLAT, gn[:, 2:3], gn, cst, tA, tB)
        steps = []
        for rho in range(16):
            po = P[2 + rho % 2]
            pd = P[4 + rho % 2]
            keys = [(0, None), (1, None)] + [(2 + kap, tb) for kap, tb in na_key_list(rho)]
            for li, (kb, tb) in enumerate(keys):
                ps = psb[ecnt % 4]
                E = Es[ecnt % 4]
                ecnt += 1
                last = li == len(keys) - 1
                st = {}
                st["S"] = (lambda ps=ps, kb=kb, rho=rho: k.mm(
                    ps[:, 0:128], KT[:, kb * 128:(kb + 1) * 128], QT[:, rho * 128:(rho + 1) * 128], True, True,
                    [KT, QT], [ps]))

                def post(ps=ps, E=E, tb=tb):
                    k.act(E[:, 0:128], ps[:, 0:128], AF.Exp, [ps], [E], scale=scale)
                    if tb is not None:
                        k.tt("dve", E[:, 0:128], E[:, 0:128], Bx[:, tb * 128:(tb + 1) * 128], ALU.mult, [E, Bx], [E])
                st["post"] = post

                def pv(po=po, pd=pd, E=E, kb=kb, first=(li == 0), last=last):
                    k.mm(po[:, 0:128], Vb[:, kb * 128:(kb + 1) * 128], E[:, 0:128], first, last, [Vb, E], [po])
                    k.mm(pd[:, 0:128], onesb[:], E[:, 0:128], first, last, [onesb, E], [pd])
                st["PV"] = pv
                if last:
                    st["after"] = (lambda po=po, pd=pd, h=h, rho=rho: finish_group(po, pd, 128, (8 + h) * NLAT + rho * 128))
                steps.append(st)
        emit_skewed(steps)


def build_a1():
    k = K()
    s01 = k.dram("s01", [2, NT, D], F32, "ExternalInput")
    xs = k.dram("xsum", [NT, D], F32, "Internal")
    pvd = k.dram("pv", [128, 160], F32, "ExternalInput")
    cstd = k.dram("cst", [128, 768], F32, "ExternalInput")
    wl = k.dram("wl", [36, 128, 4096], F32, "ExternalInput")
    gnd = k.dram("gn", [128, 4], F32, "ExternalInput")
    CS = k.dram("CS", [2, 128, 2048], F32, "ExternalInput")
    Btd = k.dram("Bt", [8, 128, 21 * 128], F32, "ExternalInput")
    wol = k.dram("wol", [8, 128, 8192], F32, "ExternalInput")
    grep = k.dram("grep", [2, 128, D], F32, "ExternalInput")
    rmd = k.dram("rm", [128, 1], F32, "ExternalInput")
    so = k.dram("so", [NLAT, D], F32, "ExternalOutput")
    zT = k.dram("zT", [36 * 128, NT], F32, "Internal")
    vtm = k.dram("vtm", [NT, 1280], F32, "Internal")
    arena = k.sbuf("arena", [128, 43500], F32)
    pv = k.sbuf("pvs", [128, 160], F32)
    cst = k.sbuf("csts", [128, 768], F32)
    gn = k.sbuf("gns", [128, 4], F32)
    rm = k.sbuf("rms", [128, 1], F32)
    P = [k.psum("P%d" % i, [128, 512]) for i in range(8)]
    for a, b_ in ((pv, pvd), (cst, cstd), (gn, gnd), (rm, rmd)):
        k.dma("sp", a[:], b_[:], writes=[a], key=a)
    tm = {18: 0, 19: 1}
    tm.update({28 + i: 2 + i for i in range(8)})
    emit_norm_proj(k, P, arena, s01[0], pv, cst, wl, 36, tm, zT, vtm, 0, xs_b=s01[1], xsum=xs)
    k.barrier()
    yT, off = k.carve(arena, 0, 16 * NLAT, BF16, "yT")
    emit_attn_cd(k, P, arena, off, zT, vtm, yT, cst, gn, CS, Btd)
    k.barrier()
    emit_wout(k, P, arena, off, yT, wol, xs, grep, rm, so, n_tb=16, n_ctx_tb=0, nt=NLAT, x_row0=NCTX)
    return k.finish()


def a1_inputs(inp, mod1, b, hf, s01, consts):
    m = mod1
    pv = np.concatenate([feat_major(inp["l1_norm1"]), feat_major(m[b, D:2 * D]), feat_major(m[b, 0:D]),
                         feat_major(m[4, D:2 * D]), feat_major(m[4, 0:D])], axis=1)
    gn = np.stack([inp["l1_gqa_q_norm"], inp["l1_gqa_k_norm"], inp["l1_na_q_norm"], inp["l1_na_k_norm"]], axis=1)
    rows = np.concatenate([np.arange(hf * 1024, (hf + 1) * 1024), 2048 + np.arange(hf * 1024, (hf + 1) * 1024)])
    grep = np.stack([np.broadcast_to(m[b, 2 * D:3 * D], (128, D)), np.broadcast_to(m[4, 2 * D:3 * D], (128, D))])
    Bt = np.stack([na_tables(inp["l1_na_rpb"][hf * 8 + h]).transpose(1, 0, 2).reshape(128, 21 * 128) for h in range(8)])
    return {"s01": s01, "pv": np.ascontiguousarray(pv), "cst": consts["cst"],
            "wl": layout_w_blocks(inp["l1_w_in"], a1_weight_cols(hf)), "gn": np.ascontiguousarray(gn.astype(np.float32)),
            "CS": consts["CS"], "Bt": np.ascontiguousarray(Bt), "wol": layout_wout(inp["l1_w_out"], rows),
            "grep": np.ascontiguousarray(grep), "rm": np.full((128, 1), 1.0 if hf == 0 else 0.0, np.float32)}


def build_c(rows):
    k = K()
    s01 = k.dram("s01", [2, rows, D], F32, "ExternalInput")
    out = k.dram("out", [rows, D], F32, "ExternalOutput")
    a = [k.sbuf("ca%d" % i, [128, D], F32) for i in range(2)]
    b_ = [k.sbuf("cb%d" % i, [128, D], F32) for i in range(2)]
    for ti in range(rows // 128):
        r = slice(ti * 128, (ti + 1) * 128)
        x, y = a[ti % 2], b_[ti % 2]
        k.dma("sp", x[:], s01[0, r, :], writes=[x], key=x)
        k.dma("pool", y[:], s01[1, r, :], writes=[y], key=y)
        k.tt("dve", x[:], x[:], y[:], ALU.add, [x, y], [x])
        k.dma("sp", out[r, :], x[:], reads=[x], key=x, is_output=True)
    return k.finish()


def _run(nc, in_maps):
    res = run_bass_kernel_spmd(nc, in_maps, core_ids=list(range(NCORES)))
    return res.results


def kernel_unfused(**inp):
    inp = {k_: np.asarray(v) for k_, v in inp.items()}
    consts = make_consts()
    B = 4
    mod = run_mod(inp)
    cores = [(b, hf) for b in range(B) for hf in range(2)]
    r = _run(build_a0(), [a0_inputs(inp, mod[0], b, hf, consts) for b, hf in cores])
    s = [x["so"] for x in r]
    in_maps = [f_inputs(inp, 0, mod[0], b, hf, np.stack([s[2 * b], s[2 * b + 1]]), consts, NCTX) for b, hf in cores]
    r = _run(build_f(NCTX), in_maps)
    s = [x["so"].reshape(NT, D) for x in r]
    in_maps = [a1_inputs(inp, mod[1], b, hf, np.stack([s[2 * b], s[2 * b + 1]]), consts) for b, hf in cores]
    r = _run(build_a1(), in_maps)
    s = [x["so"] for x in r]
    in_maps = [f_inputs(inp, 1, mod[1], b, hf, np.stack([s[2 * b], s[2 * b + 1]]), consts, 0) for b, hf in cores]
    r = _run(build_f(0), in_maps)
    s = [x["so"].reshape(NLAT, D) for x in r]
    in_maps = []
    for c in range(NCORES):
        b, half = c // 2, c % 2
        rows = slice(half * 1024, (half + 1) * 1024)
        in_maps.append({"s01": np.stack([s[2 * b][rows], s[2 * b + 1][rows]])})
    r = _run(build_c(1024), in_maps)
    out = np.stack([np.concatenate([r[2 * b]["out"], r[2 * b + 1]["out"]], axis=0) for b in range(B)])
    return out.astype(np.float32)


def emit_mod_full(k, P, arena, csT_d, mw, mb, modrow):
    off = 0
    c_sb, off = k.carve(arena, off, 256, F32, "m_c")
    s_sb, off = k.carve(arena, off, 256, F32, "m_s")
    b_sb, off = k.carve(arena, off, MCOLS, F32, "m_b")
    r_sb, off = k.carve(arena, off, MCOLS, F32, "m_r")
    wb = []
    for i in range(4):
        t, off = k.carve(arena, off, MCOLS, F32, "m_w%d" % i)
        wb.append(t)
    k.dma("sp", c_sb[:], csT_d[:], writes=[c_sb], key=c_sb)
    k.act(s_sb[:], c_sb[:], AF.Silu, [c_sb], [s_sb])
    it = 0
    for l in range(2):
        for sh in range(8):
            k.dma("pool", b_sb[0:8, :], mb[l, :, sh * MCOLS:(sh + 1) * MCOLS], writes=[b_sb], key=b_sb)
            for kt in range(32):
                w = wb[it % 4]
                it += 1
                k.dma("sp" if it % 2 else "pool", w[:], mw[l, sh, kt * 128:(kt + 1) * 128, :], writes=[w], key=w)
                for n in range(6):
                    k.mm(P[n][0:8, :], s_sb[:, kt * 8:(kt + 1) * 8], w[:, n * 512:(n + 1) * 512], kt == 0, kt == 31,
                         [s_sb, w], [P[n]], inc=(n == 5))
            for n in range(6):
                k.tt("dve", r_sb[0:8, n * 512:(n + 1) * 512], P[n][0:8, :], b_sb[0:8, n * 512:(n + 1) * 512], ALU.add,
                     [P[n], b_sb], [r_sb])
            k.dma("sp", modrow[l, :, sh * MCOLS:(sh + 1) * MCOLS], r_sb[0:8, :], reads=[r_sb], key=r_sb)


def emit_layer_mod(k, P, arena, off0, modrow, l, gains, cst, sel, pvA, pvF, grepA, grepF):
    off = off0
    mr, off = k.carve(arena, off, 4096, F32, "lm_mr")
    mT, off = k.carve(arena, off, 6 * 256, F32, "lm_mT")
    gsb, off = k.carve(arena, off, 1024, F32, "lm_g")
    for m in range(6):
        k.dma("sp", mr[0:8, :], modrow[l, :, m * D:(m + 1) * D], writes=[mr], key=mr)
        pb = P[m % 2]
        for kt in range(32):
            k.tr(pb[:, kt * 8:(kt + 1) * 8], mr[0:8, kt * 128:(kt + 1) * 128], cst[0:8, 0:8], [mr, cst], [pb],
                 inc=(kt == 31))
        k.copy("dve", mT[:, m * 256:(m + 1) * 256], pb[:, 0:256], [pb], [mT])
        if m in (2, 5):
            grep = grepA if m == 2 else grepF
            for n in range(8):
                for v in range(2):
                    pg = P[2 + v]
                    k.mm(pg[:, :], sel[0:8, v * 128:(v + 1) * 128], mr[0:8, n * 512:(n + 1) * 512], True, True,
                         [sel, mr], [pg])
                    k.copy("act", gsb[:, v * 512:(v + 1) * 512], pg[:, :], [pg], [gsb])
                    k.dma("sp", grep[v, :, n * 512:(n + 1) * 512], gsb[:, v * 512:(v + 1) * 512], reads=[gsb], key=gsb)

    def vec(m, v):
        return mT[:, m * 256 + v:(m + 1) * 256:8]
    for pv, goff, m_sh, m_sc in ((pvA, l * 64, 0, 1), (pvF, l * 64 + 32, 3, 4)):
        k.copy("dve", pv[:, 0:32], gains[:, goff:goff + 32], [gains], [pv])
        k.copy("dve", pv[:, 32:64], vec(m_sc, 0), [mT], [pv])
        k.copy("dve", pv[:, 64:96], vec(m_sh, 0), [mT], [pv])
        k.copy("dve", pv[:, 96:128], vec(m_sc, 1), [mT], [pv])
        k.copy("dve", pv[:, 128:160], vec(m_sh, 1), [mT], [pv])


def emit_f(k, P, arena, src, pv, rt, cst, ones1, w13l, w2l, grep, so2, xns, n_ctx, n_exp=16):
    ntf = n_ctx + 2048
    ntile = ntf // 128
    n_ctx_tiles = n_ctx // 128
    nch = 3 if n_ctx else 2
    NS = 256 + (32 if n_ctx else 0)
    ch_rows = [128, 128, 32][:nch]
    ch_col = [0, 128, 256][:nch]
    NE = n_exp
    so = so2.t.rearrange("(t h) c -> t (h c)", h=2)
    ident = cst[:, CST_ID]
    off = 0
    AB, off = k.carve(arena, off, 64, F32, "f_AB")
    affT, off = k.carve(arena, off, ntf, F32, "f_affT")
    sm, off = k.carve(arena, off, 8, F32, "f_sm")
    off_keep = off
    for i, sc_off in enumerate((32, 96)):
        k.stt(AB[:, i * 32:(i + 1) * 32], pv[:, sc_off:sc_off + 32], 1.0, pv[:, 0:32], ALU.add, ALU.mult, [pv], [AB])
    sh_l = pv[:, 64:96]
    sh_c = pv[:, 128:160]
    x1t, off = k.carve(arena, off, 4096, F32, "f_x1t")
    xnt, off = k.carve(arena, off, 4096, F32, "f_xnt")
    h2T, off = k.carve(arena, off, 4096, F32, "f_h2T")
    ex, off = k.carve(arena, off, 32, F32, "f_ex")
    for ti in range(ntile):
        is_ctx = ti < n_ctx_tiles
        rows = slice(ti * 128, (ti + 1) * 128)
        k.dma("sp", x1t[:], src[rows, :], writes=[x1t], key=x1t)
        k.dma("pool", so[rows, :], x1t[:], reads=[x1t], key=x1t, is_output=True)
        k.act(xnt[:], x1t[:], AF.Square, [x1t], [xnt, sm], accum_out=sm[:, 0:1])
        k.ts("dve", sm[:, 1:2], sm[:, 0:1], 1.0 / D, EPS, ALU.mult, ALU.add, [sm], [sm])
        k.act(sm[:, 2:3], sm[:, 1:2], AF.Sqrt, [sm], [sm])
        k.op("dve", lambda be: be.reciprocal(out=sm[:, 3:4], in_=sm[:, 2:3]), [sm], [sm])
        k.ts("dve", xnt[:], x1t[:], sm[:, 3:4], None, ALU.mult, None, [x1t, sm], [xnt])
        k.dma("sp", xns[rows, :], xnt[:], reads=[xnt], key=xnt)
        for bnk in range(8):
            for q in range(4):
                kt = bnk * 4 + q
                k.tr(P[bnk][:, q * 128:(q + 1) * 128], xnt[:, kt * 128:(kt + 1) * 128], ident,
                     [xnt, cst], [P[bnk]], inc=(q == 3))
            for q in range(4):
                kt = bnk * 4 + q
                a_ap = AB[:, 32 + kt:33 + kt] if is_ctx else AB[:, kt:kt + 1]
                s_ap = sh_c[:, kt:kt + 1] if is_ctx else sh_l[:, kt:kt + 1]
                k.act(h2T[:, kt * 128:(kt + 1) * 128], P[bnk][:, q * 128:(q + 1) * 128], AF.Identity,
                      [P[bnk], AB, pv], [h2T], ss=True, scale=a_ap, bias=s_ap)
        PL = P[0]
        for kt in range(32):
            k.mm(PL[:, 0:16], h2T[:, kt * 128:(kt + 1) * 128], rt[:, kt * 16:(kt + 1) * 16], kt == 0, kt == 31,
                 [h2T, rt], [PL], inc=(kt == 31))
        k.op("dve", lambda be: be.tensor_reduce(out=sm[:, 4:5], in_=PL[:, 0:16], axis=AX.X, op=ALU.max), [PL], [sm])
        k.ts("dve", sm[:, 5:6], sm[:, 4:5], -1.0, None, ALU.mult, None, [sm], [sm])
        k.act(ex[:, 0:16], PL[:, 0:16], AF.Exp, [PL, sm], [ex, sm], bias=sm[:, 5:6], accum_out=sm[:, 6:7])
        k.op("dve", lambda be: be.reciprocal(out=sm[:, 7:8], in_=sm[:, 6:7]), [sm], [sm])
        k.ts("dve", ex[:, 16:32], ex[:, 0:16], sm[:, 7:8], None, ALU.mult, None, [ex, sm], [ex])
        k.tr(P[1][0:16, 0:128], ex[:, 16:32], ident, [ex, cst], [P[1]])
        k.copy("dve", affT[0:16, ti * 128:(ti + 1) * 128], P[1][0:16, 0:128], [P[1]], [affT])
    k.barrier()
    off = off_keep
    gateT, off = k.carve(arena, off, NE * nch, F32, "f_gateT")
    idxf, off = k.carve(arena, off, NE * nch * 3, F32, "f_idxf")
    idxu, off = k.carve(arena, off, NE * nch * 3, U32, "f_idxu")
    off_tmp = off
    wk_l, off = k.carve(arena, off, 2048, F32, "f_wk_l")
    mx, off = k.carve(arena, off, NS, F32, "f_mx")
    ix, off = k.carve(arena, off, NS, U32, "f_ix")
    ixf, off = k.carve(arena, off, NS, F32, "f_ixf")
    segs = [(n_ctx, 2048, 256, 0, wk_l)]
    if n_ctx:
        wk_c, off = k.carve(arena, off, 256, F32, "f_wk_c")
        segs.append((0, 256, 32, 256, wk_c))
    for (t0, n, cap, m0, wk) in segs:
        k.copy("dve", wk[0:NE, 0:n], affT[0:NE, t0:t0 + n], [affT], [wk])
        for it in range(cap // 8):
            ms = slice(m0 + it * 8, m0 + (it + 1) * 8)
            k.op("dve", lambda be, ms=ms, wk=wk, n=n: be.max(out=mx[0:NE, ms], in_=wk[0:NE, 0:n]), [wk], [mx])
            k.op("dve", lambda be, ms=ms, wk=wk, n=n: be.max_index(
                out=ix[0:NE, ms], in_max=mx[0:NE, ms], in_values=wk[0:NE, 0:n]), [wk, mx], [ix])
            k.op("dve", lambda be, ms=ms, wk=wk, n=n: be.match_replace(
                out=wk[0:NE, 0:n], in_to_replace=mx[0:NE, ms], in_values=wk[0:NE, 0:n], imm_value=-1.0),
                [wk, mx], [wk])
        k.copy("dve", ixf[0:NE, m0:m0 + cap], ix[0:NE, m0:m0 + cap], [ix], [ixf])
        if t0:
            k.ts("dve", ixf[0:NE, m0:m0 + cap], ixf[0:NE, m0:m0 + cap], float(t0), None, ALU.add, None, [ixf], [ixf])
    W3 = 3 * NE
    for c in range(nch):
        r_ = ch_rows[c]
        cs = slice(ch_col[c], ch_col[c] + r_)
        k.tr(P[2][0:r_, 0:NE], mx[0:NE, cs], cst[0:NE, 0:NE], [mx, cst], [P[2]])
        k.copy("dve", gateT[0:r_, c * NE:(c + 1) * NE], P[2][0:r_, 0:NE], [P[2]], [gateT])
        k.tr(P[3][0:r_, 0:NE], ixf[0:NE, cs], cst[0:NE, 0:NE], [ixf, cst], [P[3]])
        b3 = c * W3
        k.copy("dve", idxf[0:r_, b3:b3 + NE], P[3][0:r_, 0:NE], [P[3]], [idxf])
        k.ts("dve", idxf[0:r_, b3 + NE:b3 + 2 * NE], idxf[0:r_, b3:b3 + NE], 2.0, None, ALU.mult, None, [idxf], [idxf])
        k.ts("dve", idxf[0:r_, b3 + 2 * NE:b3 + 3 * NE], idxf[0:r_, b3:b3 + NE], 2.0, 1.0, ALU.mult, ALU.add,
             [idxf], [idxf])
        k.copy("dve", idxu[0:r_, b3:b3 + W3], idxf[0:r_, b3:b3 + W3], [idxf], [idxu])
    k.barrier()
    off = off_tmp
    xeT, off = k.carve(arena, off, 32 * NS, BF16, "f_xeT")
    hidT, off = k.carve(arena, off, 8 * NS, BF16, "f_hidT")
    xg, wst, wbf, gt = [], [], [], []
    for i in range(2):
        t, off = k.carve(arena, off, 4096, F32, "f_xg%d" % i)
        xg.append(t)
        t, off = k.carve(arena, off, 4096, F32, "f_wst%d" % i)
        wst.append(t)
        if i == 0:
            t, off = k.carve(arena, off, 4096, F32, "f_wst2")
            wst.append(t)
        t, off = k.carve(arena, off, 4096, BF16, "f_wbf%d" % i)
        wbf.append(t)
        t, off = k.carve(arena, off, 1024, F32, "f_gt%d" % i)
        gt.append(t)
    tmp, off = k.carve(arena, off, NS, F32, "f_tmp")
    ye = []
    for i in range(nch):
        t, off = k.carve(arena, off, 2048, F32, "f_ye%d" % i)
        ye.append(t)
    so_dep = Dep("so_acc")
    gcnt = 0
    wcnt = 0
    for e in range(NE):
        for c in range(nch):
            r_ = ch_rows[c]
            g_ = xg[gcnt % 2]
            gcnt += 1
            b3 = c * W3
            k.idma(g_[0:r_, :], xns[:, :], in_offset=bass.IndirectOffsetOnAxis(ap=idxu[0:r_, b3 + e:b3 + e + 1], axis=0),
                   reads=[idxu], writes=[g_], key=g_)
            is_ctx = (c == 2)
            for bq in range(8):
                bank = P[bq % 4]
                for q in range(4):
                    kt = bq * 4 + q
                    k.tr(bank[:, q * 128:q * 128 + r_], g_[0:r_, kt * 128:(kt + 1) * 128], cst[0:r_, 0:r_],
                         [g_, cst], [bank], inc=(q == 3))
                for q in range(4):
                    kt = bq * 4 + q
                    a_ap = AB[:, 32 + kt:33 + kt] if is_ctx else AB[:, kt:kt + 1]
                    s_ap = sh_c[:, kt:kt + 1] if is_ctx else sh_l[:, kt:kt + 1]
                    k.act(xeT[:, kt * NS + ch_col[c]:kt * NS + ch_col[c] + r_], bank[:, q * 128:q * 128 + r_],
                          AF.Identity, [bank, AB, pv], [xeT], ss=True, scale=a_ap, bias=s_ap)
        for fb in range(8):
            pbs = []
            for which in range(2):
                ws = wst[wcnt % 3]
                wb = wbf[wcnt % 2]
                k.dma("sp" if wcnt % 2 == 0 else "pool", ws[:], w13l[e, which, fb], writes=[ws], key=ws)
                k.copy("dve" if wcnt % 2 == 0 else "act", wb[:], ws[:], [ws], [wb])
                wcnt += 1
                pb = P[4 + which]
                for kt in range(32):
                    k.mm(pb[:, 0:NS], wb[:, kt * 128:(kt + 1) * 128], xeT[:, kt * NS:(kt + 1) * NS],
                         kt == 0, kt == 31, [wb, xeT], [pb], inc=(kt == 31))
                pbs.append(pb)
            k.act(tmp[:], pbs[0][:, 0:NS], AF.Silu, [pbs[0]], [tmp])
            k.tt("dve", hidT[:, fb * NS:(fb + 1) * NS], tmp[:], pbs[1][:, 0:NS], ALU.mult, [tmp, pbs[1]], [hidT])
        for n in range(8):
            ws = wst[wcnt % 3]
            wb = wbf[wcnt % 2]
            g = gt[n % 2]
            k.dma("sp" if wcnt % 2 == 0 else "pool", ws[:], w2l[e, n], writes=[ws], key=ws)
            k.copy("dve" if wcnt % 2 == 0 else "act", wb[:], ws[:], [ws], [wb])
            wcnt += 1
            k.dma("sp", g[:, 0:512], grep[0, :, n * 512:(n + 1) * 512], writes=[g], key=g)
            k.dma("sp", g[:, 512:1024], grep[1, :, n * 512:(n + 1) * 512], writes=[g], key=g)
            hcol = n // 4
            for c in range(nch):
                r_ = ch_rows[c]
                pb = P[6 + (n * nch + c) % 2]
                for fb in range(8):
                    k.mm(pb[0:r_, :], hidT[:, fb * NS + ch_col[c]:fb * NS + ch_col[c] + r_],
                         wb[:, fb * 512:(fb + 1) * 512], fb == 0, fb == 7, [hidT, wb], [pb], inc=(fb == 7))
                y_ = ye[c]
                gs = g[0:r_, 512:1024] if c == 2 else g[0:r_, 0:512]
                k.stt(y_[0:r_, (n % 4) * 512:(n % 4 + 1) * 512], pb[0:r_, :], gateT[0:r_, c * NE + e:c * NE + e + 1], gs,
                      ALU.mult, ALU.mult, [pb, gateT, g], [y_])
            if n % 4 == 3:
                for c in range(nch):
                    r_ = ch_rows[c]
                    y_ = ye[c]
                    b3 = c * W3 + NE + hcol * NE
                    k.idma(so2[:, :], y_[0:r_, :], out_offset=bass.IndirectOffsetOnAxis(
                        ap=idxu[0:r_, b3 + e:b3 + e + 1], axis=0),
                        reads=[y_, idxu], writes=[so_dep], key=so_dep, add=True, is_output=True)


def build_fused(upto=9):
    k = K()
    def kind(name, lvl):
        return "ExternalOutput" if upto == lvl else "Internal"
    xs = k.dram("xs", [NT, D], F32, "ExternalInput")
    csT = k.dram("csT", [128, 256], F32, "ExternalInput")
    mw = k.dram("mw", [2, 8, 4096, MCOLS], F32, "ExternalInput")
    mb = k.dram("mb", [2, 8, 6 * D], F32, "ExternalInput")
    gainsd = k.dram("gains", [128, 128], F32, "ExternalInput")
    cstd = k.dram("cst", [128, 768], F32, "ExternalInput")
    seld = k.dram("sel", [8, 256], F32, "ExternalInput")
    CS = k.dram("CS", [2, 128, 2048], F32, "ExternalInput")
    wl0 = k.dram("wl0", [96, 128, 4096], F32, "ExternalInput") if upto >= 3 else None
    wol0 = k.dram("wol0", [2, 8, 128, 8192], F32, "ExternalInput") if upto >= 3 else None
    pl0 = k.dram("pl0", [2, 128, 88], F32, "ExternalInput") if upto >= 3 else None
    wa0 = k.dram("wa0", [2, 128, 2048], F32, "ExternalInput") if upto >= 3 else None
    wx0 = k.dram("wx0", [2, 128, 2048], F32, "ExternalInput") if upto >= 3 else None
    dec0 = k.dram("dec0", [2, 128, 8 * DEC_W], F32, "ExternalInput") if upto >= 3 else None
    rt0 = k.dram("rt0", [128, 512], F32, "ExternalInput") if upto >= 4 else None
    w13l0 = k.dram("w13l0", [16, 2, 8, 128, 4096], F32, "ExternalInput") if upto >= 4 else None
    w2l0 = k.dram("w2l0", [16, 8, 128, 4096], F32, "ExternalInput") if upto >= 4 else None
    wl1 = k.dram("wl1", [72, 128, 4096], F32, "ExternalInput") if upto >= 6 else None
    wol1 = k.dram("wol1", [2, 8, 128, 8192], F32, "ExternalInput") if upto >= 6 else None
    gnd = k.dram("gn", [128, 4], F32, "ExternalInput")
    Btd = k.dram("Bt", [2, 8, 128, 21 * 128], F32, "ExternalInput") if upto >= 6 else None
    rt1 = k.dram("rt1", [128, 512], F32, "ExternalInput") if upto >= 7 else None
    w13l1 = k.dram("w13l1", [16, 2, 8, 128, 4096], F32, "ExternalInput") if upto >= 7 else None
    w2l1 = k.dram("w2l1", [16, 8, 128, 4096], F32, "ExternalInput") if upto >= 7 else None
    out2 = k.dram("out", [NLAT * 2, 2048], F32, "ExternalOutput") if upto >= 7 else None
    modrow = k.dram("modrow", [2, 8, 6 * D], F32, kind("modrow", 1))
    grepA = k.dram("grepA", [2, 128, D], F32, "Internal")
    grepF = k.dram("grepF", [2, 128, D], F32, "Internal")
    zT = k.dram("zT", [96 * 128, NT], F32, "Internal")
    vtm = k.dram("vtm", [NT, 2560], F32, "Internal")
    S1 = k.dram("S1", [NT, D], F32, kind("S1", 3))
    S2 = k.dram("S2", [NT * 2, 2048], F32, kind("S2", 4))
    S3 = k.dram("S3", [NLAT, D], F32, kind("S3", 6))
    xns = k.dram("xns", [NT, D], F32, "Internal")
    arena = k.sbuf("arena", [128, 43500], F32)
    cst = k.sbuf("csts", [128, 768], F32)
    sel = k.sbuf("sels", [8, 256], F32)
    gains = k.sbuf("gainss", [128, 128], F32)
    pvA = k.sbuf("pvA", [128, 160], F32)
    pvF = k.sbuf("pvF", [128, 160], F32)
    pl = k.sbuf("pls", [128, 88], F32)
    cl = k.sbuf("cl", [128, 16], F32)
    ones1 = k.sbuf("ones1", [128, 1], F32)
    gn = k.sbuf("gns", [128, 4], F32)
    rt = k.sbuf("rts", [128, 512], F32)
    P = [k.psum("P%d" % i, [128, 512]) for i in range(8)]
    for a, b_ in ((cst, cstd), (sel, seld), (gains, gainsd), (gn, gnd)):
        k.dma("sp", a[:], b_[:], writes=[a], key=a)
    k.memset("dve", ones1[:], 1.0, [ones1])
    S2v = S2.t.rearrange("(t h) c -> t (h c)", h=2)

    emit_mod_full(k, P, arena, csT, mw, mb, modrow)
    k.barrier()
    if upto == 1:
        return k.finish()
    emit_layer_mod(k, P, arena, 0, modrow, 0, gains, cst, sel, pvA, pvF, grepA, grepF)
    k.barrier()
    tm0 = {32 + i: i for i in range(8)}
    tm0.update({48 + 32 + i: 8 + i for i in range(8)})
    emit_norm_proj(k, P, arena, xs, pvA, cst, wl0, 96, tm0, zT, vtm, 0)
    k.barrier()
    for hf in range(2):
        zr = hf * 48 * 128
        yT, off = k.carve(arena, 0, 16 * NT, BF16, "yT")
        wa, o2 = k.carve(arena, off + 9 * NT, 2048, F32, "wa")
        wx, o2 = k.carve(arena, o2, 2048, F32, "wx")
        k.dma("sp", pl[:], pl0[hf], writes=[pl], key=pl)
        k.dma("sp", wa[:], wa0[hf], writes=[wa], key=wa)
        k.dma("pool", wx[:], wx0[hf], writes=[wx], key=wx)
        emit_lru(k, P, arena, off, zT, yT, pl, wa, wx, cl, zrow0=zr)
        k.barrier()
        dec, o3 = k.carve(arena, off, 8 * DEC_W, F32, "dec")
        k.dma("sp", dec[:], dec0[hf], writes=[dec], key=dec)
        emit_ret(k, P, arena, o3, zT, vtm, yT, cst, dec, zr + 2048, zr + 3072, zr + 5120, CS, vcol0=hf * 1024)
        k.barrier()
        emit_wout(k, P, arena, off, yT, wol0[hf], xs if hf == 0 else S1, grepA, ones1, S1)
        k.barrier()
    if upto == 3:
        return k.finish()
    k.dma("sp", rt[:], rt0[:], writes=[rt], key=rt)
    emit_f(k, P, arena, S1, pvF, rt, cst, ones1, w13l0, w2l0, grepF, S2, xns, NCTX)
    k.barrier()
    if upto == 4:
        return k.finish()
    emit_layer_mod(k, P, arena, 0, modrow, 1, gains, cst, sel, pvA, pvF, grepA, grepF)
    k.barrier()
    tm1 = {18: 0, 19: 1}
    tm1.update({28 + i: 2 + i for i in range(8)})
    tm1.update({36 + 18: 10, 36 + 19: 11})
    tm1.update({36 + 28 + i: 12 + i for i in range(8)})
    emit_norm_proj(k, P, arena, S2v, pvA, cst, wl1, 72, tm1, zT, vtm, 0)
    k.barrier()
    for hf in range(2):
        yT, off = k.carve(arena, 0, 16 * NLAT, BF16, "yT1")
        emit_attn_cd(k, P, arena, off, zT, vtm, yT, cst, gn, CS, Btd[hf], zb0=hf * 36, vidx0=hf * 10)
        k.barrier()
        emit_wout(k, P, arena, off, yT, wol1[hf], S2v if hf == 0 else S3, grepA, ones1, S3,
                  n_tb=16, n_ctx_tb=0, nt=NLAT, x_row0=NCTX if hf == 0 else 0)
        k.barrier()
    if upto == 6:
        return k.finish()
    k.dma("sp", rt[:], rt1[:], writes=[rt], key=rt)
    emit_f(k, P, arena, S3, pvF, rt, cst, ones1, w13l1, w2l1, grepF, out2, xns, 0)
    return k.finish()


def fused_shared_inputs(inp):
    sh = {}
    sh["mw"] = np.stack([np.stack([np.ascontiguousarray(inp["l%d_mod_w" % l][:, s * MCOLS:(s + 1) * MCOLS])
                                   for s in range(8)]) for l in range(2)])
    sh["mb"] = np.ascontiguousarray(np.stack([np.broadcast_to(inp["l%d_mod_b" % l], (8, 6 * D)) for l in range(2)]))
    sh["gains"] = np.ascontiguousarray(np.concatenate(
        [feat_major(inp["l0_norm1"]), feat_major(inp["l0_norm2"]), feat_major(inp["l1_norm1"]),
         feat_major(inp["l1_norm2"])], axis=1))
    sel = np.zeros((8, 256), np.float32)
    sel[0, 0:128] = 1.0
    sel[1, 128:256] = 1.0
    sh["sel"] = sel
    consts = make_consts()
    sh["cst"] = consts["cst"]
    sh["CS"] = consts["CS"]
    rows = [np.concatenate([np.arange(hf * 1024, (hf + 1) * 1024), 2048 + np.arange(hf * 1024, (hf + 1) * 1024)])
            for hf in range(2)]
    sh["wl0"] = np.concatenate([layout_w_blocks(inp["l0_w_in"], a0_weight_cols(hf)) for hf in range(2)])
    sh["wol0"] = np.stack([layout_wout(inp["l0_w_out"], rows[hf]) for hf in range(2)])
    lp = [lru_params(inp, hf) for hf in range(2)]
    sh["pl0"] = np.stack([x[0] for x in lp])
    sh["wa0"] = np.stack([x[1] for x in lp])
    sh["wx0"] = np.stack([x[2] for x in lp])
    sh["dec0"] = np.stack([ret_dec_table(hf) for hf in range(2)])
    ar = np.arange(1024)
    for l in range(2):
        p = "l%d_" % l
        r = inp[p + "router"]
        sh["rt%d" % l] = np.ascontiguousarray(r.reshape(32, 128, 16).transpose(1, 0, 2).reshape(128, 512))
        sh["w13l%d" % l] = np.stack([np.stack([layout_w_blocks(inp[p + "exp_w1"][e], ar),
                                               layout_w_blocks(inp[p + "exp_w3"][e], ar)]) for e in range(16)])
        w2 = inp[p + "exp_w2"]
        sh["w2l%d" % l] = np.ascontiguousarray(
            w2.reshape(16, 8, 128, 8, 512).transpose(0, 3, 2, 1, 4).reshape(16, 8, 128, 4096))
    sh["wl1"] = np.concatenate([layout_w_blocks(inp["l1_w_in"], a1_weight_cols(hf)) for hf in range(2)])
    sh["wol1"] = np.stack([layout_wout(inp["l1_w_out"], rows[hf]) for hf in range(2)])
    gn = np.stack([inp["l1_gqa_q_norm"], inp["l1_gqa_k_norm"], inp["l1_na_q_norm"], inp["l1_na_k_norm"]], axis=1)
    sh["gn"] = np.ascontiguousarray(gn.astype(np.float32))
    sh["Bt"] = np.ascontiguousarray(np.stack([np.stack(
        [na_tables(inp["l1_na_rpb"][hf * 8 + h]).transpose(1, 0, 2).reshape(128, 21 * 128) for h in range(8)])
        for hf in range(2)]))
    return sh


def fused_core_inputs(inp, b, sh):
    cs = np.zeros((8, D), np.float32)
    cs[0] = inp["c"][b]
    cs[1] = inp["c_ctx"]
    m = dict(sh)
    m["csT"] = np.ascontiguousarray(cs.T.reshape(32, 128, 8).transpose(1, 0, 2).reshape(128, 256))
    m["xs"] = np.concatenate([inp["ctx"][b], inp["x"][b]], axis=0)
    return m


def kernel(**inp):
    inp = {k_: np.asarray(v) for k_, v in inp.items()}
    sh = fused_shared_inputs(inp)
    nc = build_fused()
    in_maps = [fused_core_inputs(inp, b, sh) for b in range(4)]
    res = run_bass_kernel_spmd(nc, in_maps, core_ids=list(range(4)))
    return np.stack([r["out"].reshape(NLAT, D) for r in res.results]).astype(np.float32)
```
